# Optimizing a Trainium2 kernel written in Bass

```python
import math
import jax, jax.numpy as jnp
from jax import lax
import numpy as np

D_MODEL = 1024
BATCH = 2
SEQ = 8192
DEPTH = 2

N_MEM = 256
N_A_LAYERS = DEPTH // 2
N_B_LAYERS = DEPTH - N_A_LAYERS
HEAD_DIM = 64
FOX_HEADS = 12
MEM_HEADS = 4
MLA_HEADS = 12
Q_LORA = 384
KV_LORA = 256
QK_NOPE = 64
QK_ROPE = 32
V_DIM = 64
ROPE_THETA = 10000.0
N_EXPERTS = 32
TOP_K = 4
D_EXPERT = D_MODEL
SWIGLU_LIMIT = 7.0
SWIGLU_ALPHA = 1.702
MOE_BLOCK = 128
Q_BLOCK = 128
LN_EPS = 1e-5
RMS_EPS = 1e-6
NEG_INF = -1e30
DEEPNORM_ALPHA = (2 * DEPTH) ** 0.25
DEEPNORM_BETA = (8 * DEPTH) ** -0.25
FOX_WIDTH = FOX_HEADS * HEAD_DIM
MEM_WIDTH = MEM_HEADS * HEAD_DIM
MLA_V_WIDTH = MLA_HEADS * V_DIM
FOX_IN = 3 * FOX_WIDTH + FOX_HEADS + MEM_WIDTH
MLA_IN = Q_LORA + MEM_WIDTH
MIX_OUT_A = FOX_WIDTH + MEM_WIDTH
MIX_OUT_B = MLA_V_WIDTH + MEM_WIDTH

kernel_name = 'hybrid_fox_mla_yoco_moe_block'


def layer_norm(x, g, b):
    xf = x.astype(jnp.float32)
    mu = jnp.mean(xf, axis=-1, keepdims=True)
    var = jnp.mean(jnp.square(xf - mu), axis=-1, keepdims=True)
    y = (xf - mu) * lax.rsqrt(var + LN_EPS) * g.astype(jnp.float32) + b.astype(jnp.float32)
    return y.astype(x.dtype)


def rms_norm(x, g):
    xf = x.astype(jnp.float32)
    y = xf * lax.rsqrt(jnp.mean(jnp.square(xf), axis=-1, keepdims=True) + RMS_EPS) * g.astype(jnp.float32)
    return y.astype(x.dtype)


def rope(x, positions):
    r = x.shape[-1]
    half = r // 2
    inv_freq = ROPE_THETA ** (-jnp.arange(half, dtype=jnp.float32) * 2.0 / r)
    ang = positions.astype(jnp.float32)[..., None] * inv_freq
    cos = jnp.cos(ang)[:, :, None, :]
    sin = jnp.sin(ang)[:, :, None, :]
    xf = x.astype(jnp.float32)
    x1, x2 = xf[..., :half], xf[..., half:]
    return jnp.concatenate([x1 * cos - x2 * sin, x1 * sin + x2 * cos], axis=-1).astype(x.dtype)


def causal_block_attention(q, k, v, scale, cum=None):
    B, S, H, Dk = q.shape
    nb = S // Q_BLOCK
    qb = q.reshape(B, nb, Q_BLOCK, H, Dk).swapaxes(0, 1)
    key_pos = jnp.arange(S)
    blk_idx = jnp.arange(nb)
    if cum is not None:
        cum_k = cum.transpose(0, 2, 1)
        cum_q = cum.reshape(B, nb, Q_BLOCK, H).swapaxes(0, 1)

    def one_block(args):
        i, qi = args[0], args[1]
        s = jnp.einsum('bqhd,bkhd->bhqk', qi, k, preferred_element_type=jnp.float32) * scale
        if cum is not None:
            ci = args[2]
            s = s + ci.transpose(0, 2, 1)[..., None] - cum_k[:, :, None, :]
        q_pos = i * Q_BLOCK + jnp.arange(Q_BLOCK)
        s = jnp.where(key_pos[None, :] <= q_pos[:, None], s, NEG_INF)
        p = jax.nn.softmax(s, axis=-1)
        return jnp.einsum('bhqk,bkhd->bqhd', p.astype(v.dtype), v)

    xs = (blk_idx, qb) if cum is None else (blk_idx, qb, cum_q)
    out = lax.map(one_block, xs)
    return out.swapaxes(0, 1).reshape(B, S, H, v.shape[-1])


def memory_attention(q_flat, mem, w_mkv):
    B, S, _ = q_flat.shape
    M = mem.shape[1]
    q = q_flat.reshape(B, S, MEM_HEADS, HEAD_DIM)
    mkv = jnp.einsum('bmd,de->bme', mem, w_mkv)
    mk = mkv[..., :MEM_WIDTH].reshape(B, M, MEM_HEADS, HEAD_DIM)
    mv = mkv[..., MEM_WIDTH:].reshape(B, M, MEM_HEADS, HEAD_DIM)
    s = jnp.einsum('bshd,bmhd->bhsm', q, mk, preferred_element_type=jnp.float32) * (HEAD_DIM ** -0.5)
    p = jax.nn.softmax(s, axis=-1)
    o = jnp.einsum('bhsm,bmhd->bshd', p.astype(mv.dtype), mv)
    return o.reshape(B, S, MEM_WIDTH)


def fox_mixer(x, mem, w_in, b_f, w_out, w_mkv):
    B, S, _ = x.shape
    proj = jnp.einsum('bsd,de->bse', x, w_in)
    q = proj[..., :FOX_WIDTH].reshape(B, S, FOX_HEADS, HEAD_DIM)
    k = proj[..., FOX_WIDTH:2 * FOX_WIDTH].reshape(B, S, FOX_HEADS, HEAD_DIM)
    v = proj[..., 2 * FOX_WIDTH:3 * FOX_WIDTH].reshape(B, S, FOX_HEADS, HEAD_DIM)
    f_logit = proj[..., 3 * FOX_WIDTH:3 * FOX_WIDTH + FOX_HEADS]
    mq = proj[..., 3 * FOX_WIDTH + FOX_HEADS:]
    log_f = jax.nn.log_sigmoid(f_logit.astype(jnp.float32) + b_f.astype(jnp.float32))
    cum = jnp.cumsum(log_f, axis=1)
    fox = causal_block_attention(q, k, v, HEAD_DIM ** -0.5, cum)
    memo = memory_attention(mq, mem, w_mkv)
    merged = jnp.concatenate([fox.reshape(B, S, FOX_WIDTH), memo], axis=-1)
    return jnp.einsum('bse,ed->bsd', merged, w_out)


def shared_mla_kv(h, positions, w_dkv, g_kv, w_ukv):
    B, S, _ = h.shape
    lat = jnp.einsum('bsd,de->bse', h, w_dkv)
    c_kv = rms_norm(lat[..., :KV_LORA], g_kv)
    k_r = lat[..., KV_LORA:]
    kv = jnp.einsum('bsc,ce->bse', c_kv, w_ukv).reshape(B, S, MLA_HEADS, QK_NOPE + V_DIM)
    k_nope, v = kv[..., :QK_NOPE], kv[..., QK_NOPE:]
    k_rope = rope(k_r[:, :, None, :], positions)
    k = jnp.concatenate([k_nope, jnp.broadcast_to(k_rope, (B, S, MLA_HEADS, QK_ROPE))], axis=-1)
    return k, v


def mla_mixer(x, mem, positions, k, v, w_in, g_q, w_uq, w_out, w_mkv):
    B, S, _ = x.shape
    proj = jnp.einsum('bsd,de->bse', x, w_in)
    c_q = rms_norm(proj[..., :Q_LORA], g_q)
    mq = proj[..., Q_LORA:]
    q = jnp.einsum('bsc,ce->bse', c_q, w_uq).reshape(B, S, MLA_HEADS, QK_NOPE + QK_ROPE)
    q = jnp.concatenate([q[..., :QK_NOPE], rope(q[..., QK_NOPE:], positions)], axis=-1)
    att = causal_block_attention(q, k, v, (QK_NOPE + QK_ROPE) ** -0.5)
    memo = memory_attention(mq, mem, w_mkv)
    merged = jnp.concatenate([att.reshape(B, S, MLA_V_WIDTH), memo], axis=-1)
    return jnp.einsum('bse,ed->bsd', merged, w_out)


def moe_ffn(x, w_r, b_r, w_gu, b_gu, w_dn, b_dn):
    B, S, D = x.shape
    T = B * S
    xt = x.reshape(T, D)
    logits = jnp.dot(xt, w_r, preferred_element_type=jnp.float32) + b_r.astype(jnp.float32)
    top_logit, top_idx = lax.top_k(logits, TOP_K)
    gates = jax.nn.softmax(top_logit, axis=-1)
    M = T * TOP_K
    expert_flat = top_idx.reshape(M)
    token_flat = jnp.arange(M, dtype=jnp.int32) // TOP_K
    gate_flat = gates.reshape(M)
    order = jnp.argsort(expert_flat)
    e_sorted = expert_flat[order]
    tok_sorted = token_flat[order]
    gate_sorted = gate_flat[order]
    counts = jnp.bincount(expert_flat, length=N_EXPERTS)
    padded = ((counts + MOE_BLOCK - 1) // MOE_BLOCK) * MOE_BLOCK
    start = jnp.cumsum(counts) - counts
    pend = jnp.cumsum(padded)
    pstart = pend - padded
    dest = pstart[e_sorted] + (jnp.arange(M) - start[e_sorted])
    n_blocks = -(-M // MOE_BLOCK) + N_EXPERTS
    P = n_blocks * MOE_BLOCK
    slot_tok = jnp.full((P,), T, jnp.int32).at[dest].set(tok_sorted)
    x_pad = jnp.concatenate([xt, jnp.zeros((1, D), xt.dtype)], axis=0)
    xb = x_pad[slot_tok].reshape(n_blocks, MOE_BLOCK, D)
    block_expert = jnp.minimum(
        jnp.searchsorted(pend, jnp.arange(n_blocks) * MOE_BLOCK, side='right'), N_EXPERTS - 1)

    def expert_block(args):
        xi, e = args
        gu = jnp.dot(xi, w_gu[e]) + b_gu[e]
        g, u = gu[:, :D_EXPERT], gu[:, D_EXPERT:]
        g = jnp.minimum(g, SWIGLU_LIMIT)
        u = jnp.clip(u, -SWIGLU_LIMIT, SWIGLU_LIMIT)
        h = (u + 1.0) * (g * jax.nn.sigmoid(SWIGLU_ALPHA * g))
        return jnp.dot(h, w_dn[e]) + b_dn[e]

    yb = lax.map(expert_block, (xb, block_expert)).reshape(P, D)
    y_sorted = yb[dest].astype(jnp.float32) * gate_sorted[:, None]
    out = jax.ops.segment_sum(y_sorted, tok_sorted, num_segments=T)
    return out.astype(x.dtype).reshape(B, S, D)


def setup_inputs(seed: int = 0) -> dict:
    key = jax.random.key(seed)
    ks = jax.random.split(key, 24)

    def nrm(k, shape, scale):
        return jax.random.normal(k, shape, jnp.float32) * scale

    x = nrm(ks[0], (BATCH, SEQ, D_MODEL), 1.0)
    mem = nrm(ks[1], (BATCH, N_MEM, D_MODEL), 1.0)
    positions = jnp.broadcast_to(jnp.arange(SEQ, dtype=jnp.int32)[None, :], (BATCH, SEQ))
    a_w_in = nrm(ks[2], (N_A_LAYERS, D_MODEL, FOX_IN), D_MODEL ** -0.5)
    a_w_in = a_w_in.at[:, :, 2 * FOX_WIDTH:3 * FOX_WIDTH].multiply(DEEPNORM_BETA)
    a_b_f = nrm(ks[3], (N_A_LAYERS, FOX_HEADS), 0.1)
    a_w_out = nrm(ks[4], (N_A_LAYERS, MIX_OUT_A, D_MODEL), MIX_OUT_A ** -0.5 * DEEPNORM_BETA)
    b_w_in = nrm(ks[5], (N_B_LAYERS, D_MODEL, MLA_IN), D_MODEL ** -0.5)
    b_g_q = 1.0 + nrm(ks[6], (N_B_LAYERS, Q_LORA), 0.01)
    b_w_uq = nrm(ks[7], (N_B_LAYERS, Q_LORA, MLA_HEADS * (QK_NOPE + QK_ROPE)), Q_LORA ** -0.5)
    b_w_out = nrm(ks[8], (N_B_LAYERS, MIX_OUT_B, D_MODEL), MIX_OUT_B ** -0.5 * DEEPNORM_BETA)
    kv_w_dkv = nrm(ks[9], (D_MODEL, KV_LORA + QK_ROPE), D_MODEL ** -0.5)
    kv_g = 1.0 + nrm(ks[10], (KV_LORA,), 0.01)
    w_uk = nrm(ks[11], (KV_LORA, MLA_HEADS, QK_NOPE), KV_LORA ** -0.5)
    w_uv = nrm(ks[12], (KV_LORA, MLA_HEADS, V_DIM), KV_LORA ** -0.5 * DEEPNORM_BETA)
    kv_w_ukv = jnp.concatenate([w_uk, w_uv], axis=-1).reshape(KV_LORA, MLA_HEADS * (QK_NOPE + V_DIM))
    mem_w_kv = nrm(ks[13], (DEPTH, D_MODEL, 2 * MEM_WIDTH), D_MODEL ** -0.5)
    mem_w_kv = mem_w_kv.at[:, :, MEM_WIDTH:].multiply(DEEPNORM_BETA)
    ln_g = 1.0 + nrm(ks[14], (DEPTH, 2, D_MODEL), 0.01)
    ln_b = nrm(ks[15], (DEPTH, 2, D_MODEL), 0.01)
    moe_w_r = nrm(ks[16], (DEPTH, D_MODEL, N_EXPERTS), D_MODEL ** -0.5)
    moe_b_r = nrm(ks[17], (DEPTH, N_EXPERTS), 0.01)
    moe_w_gu = nrm(ks[18], (DEPTH, N_EXPERTS, D_MODEL, 2 * D_EXPERT), D_MODEL ** -0.5)
    moe_b_gu = nrm(ks[19], (DEPTH, N_EXPERTS, 2 * D_EXPERT), 0.01)
    moe_w_dn = nrm(ks[20], (DEPTH, N_EXPERTS, D_EXPERT, D_MODEL), D_EXPERT ** -0.5 * DEEPNORM_BETA)
    moe_b_dn = nrm(ks[21], (DEPTH, N_EXPERTS, D_MODEL), 0.01)
    return {'x': x, 'mem': mem, 'positions': positions,
            'a_w_in': a_w_in, 'a_b_f': a_b_f, 'a_w_out': a_w_out,
            'b_w_in': b_w_in, 'b_g_q': b_g_q, 'b_w_uq': b_w_uq, 'b_w_out': b_w_out,
            'kv_w_dkv': kv_w_dkv, 'kv_g': kv_g, 'kv_w_ukv': kv_w_ukv,
            'mem_w_kv': mem_w_kv, 'ln_g': ln_g, 'ln_b': ln_b,
            'moe_w_r': moe_w_r, 'moe_b_r': moe_b_r, 'moe_w_gu': moe_w_gu, 'moe_b_gu': moe_b_gu,
            'moe_w_dn': moe_w_dn, 'moe_b_dn': moe_b_dn}


def reference(x, mem, positions, a_w_in, a_b_f, a_w_out, b_w_in, b_g_q, b_w_uq, b_w_out,
              kv_w_dkv, kv_g, kv_w_ukv, mem_w_kv, ln_g, ln_b,
              moe_w_r, moe_b_r, moe_w_gu, moe_b_gu, moe_w_dn, moe_b_dn):
    shared_k = None
    shared_v = None
    for l in range(DEPTH):
        if l < N_A_LAYERS:
            mix = fox_mixer(x, mem, a_w_in[l], a_b_f[l], a_w_out[l], mem_w_kv[l])
        else:
            if shared_k is None:
                shared_k, shared_v = shared_mla_kv(x, positions, kv_w_dkv, kv_g, kv_w_ukv)
            b = l - N_A_LAYERS
            mix = mla_mixer(x, mem, positions, shared_k, shared_v,
                            b_w_in[b], b_g_q[b], b_w_uq[b], b_w_out[b], mem_w_kv[l])
        x = layer_norm(DEEPNORM_ALPHA * x + mix, ln_g[l, 0], ln_b[l, 0])
        ffn = moe_ffn(x, moe_w_r[l], moe_b_r[l], moe_w_gu[l], moe_b_gu[l], moe_w_dn[l], moe_b_dn[l])
        x = layer_norm(DEEPNORM_ALPHA * x + ffn, ln_g[l, 1], ln_b[l, 1])
    return x
```

```python
import math
import numpy as np
import concourse.bass as bass
import concourse.mybir as mybir
from concourse.bass_utils import run_bass_kernel_spmd

F32 = mybir.dt.float32
BF16 = mybir.dt.bfloat16
I32 = mybir.dt.int32
U32 = mybir.dt.uint32
AF = mybir.ActivationFunctionType
ALU = mybir.AluOpType
AX = mybir.AxisListType

S = 8192
D = 1024
NCORES = 8


class Buf:
    def __init__(self, name):
        self.name = name
        self.writers = []
        self.readers = []
        self.dsem = None
        self.dcount = 0
        self.dgroup = None


class Eng:
    def __init__(self, name, h, sem):
        self.name = name
        self.h = h
        self.sem = sem
        self.n = 0
        self.seen = {}


class Prog:
    def __init__(self, nc):
        self.nc = nc
        self.E = {}
        for name, h in (("pe", nc.tensor), ("act", nc.scalar), ("dve", nc.vector),
                        ("pool", nc.gpsimd), ("sp", nc.sync)):
            self.E[name] = Eng(name, h, nc.alloc_semaphore(name=f"sem_{name}"))
        self.nbuf = 0
        self.dma_bufs = []

    def barrier(self):
        for E in self.E.values():
            for X in self.E.values():
                if X is not E and X.n > 0:
                    self._wait_ev(E, (X.name, X.sem, X.n))
            for b in self.dma_bufs:
                if b.dcount:
                    self._wait_ev(E, ("d_" + b.name, b.dsem, b.dcount))

    def buf(self, name=None):
        self.nbuf += 1
        return Buf(f"{name or 'b'}{self.nbuf}")

    def _wait_ev(self, E, ev, same_ok=False):
        key, sem, val = ev
        if key == E.name and same_ok:
            return
        if E.seen.get(key, 0) >= val:
            return
        E.h.wait_ge(sem, val)
        E.seen[key] = val

    def _deps(self, E, reads, writes, same_ok=False, accumulate=False):
        for b in reads:
            for ev in b.writers:
                self._wait_ev(E, ev, same_ok)
        for b in writes:
            for ev in b.writers:
                self._wait_ev(E, ev, same_ok or (accumulate and ev[0] == E.name))
            for ev in b.readers:
                self._wait_ev(E, ev, same_ok)

    def _record(self, ev, reads, writes, accumulate=False):
        for b in reads:
            b.readers.append(ev)
            if len(b.readers) > 64:
                b.readers = b.readers[-64:] if False else b.readers
        for b in writes:
            if accumulate:
                b.writers.append(ev)
            else:
                b.writers = [ev]
                b.readers = []

    def op(self, eng, fn, reads=(), writes=(), accumulate=False):
        E = self.E[eng]
        self._deps(E, reads, writes, same_ok=(eng == "pe"), accumulate=accumulate)
        ins = fn(E.h)
        E.n += 1
        ins.then_inc(E.sem, 1)
        ev = (E.name, E.sem, E.n)
        self._record(ev, reads, writes, accumulate)
        for b in reads:
            last = {}
            for r in b.readers:
                last[r[0]] = r
            b.readers = list(last.values())
        for b in writes:
            if accumulate:
                last = {}
                for r in b.writers:
                    last[r[0]] = r
                b.writers = list(last.values())
        return ins

    def dma(self, q, out, in_, owner, reads=(), writes=(), group=None, accumulate=False,
            fn=None, **kw):
        Q = self.E[q]
        if owner.dsem is None:
            owner.dsem = self.nc.alloc_semaphore(name=f"dsem_{owner.name}")
            self.dma_bufs.append(owner)
        self._deps(Q, reads, writes, accumulate=accumulate)
        key = "d_" + owner.name
        if not (group is not None and owner.dgroup == group):
            if owner.dcount > 0:
                self._wait_ev(Q, (key, owner.dsem, owner.dcount))
        owner.dgroup = group
        if fn is not None:
            ins = fn(Q.h)
        else:
            ins = Q.h.dma_start(out=out, in_=in_, **kw)
        owner.dcount += 16
        ins.then_inc(owner.dsem, 16)
        ev = (key, owner.dsem, owner.dcount)
        self._record(ev, reads, writes, accumulate)
        for b in list(reads):
            last = {}
            for r in b.readers:
                last[r[0]] = r
            b.readers = list(last.values())
        for b in list(writes):
            last = {}
            for r in b.writers:
                last[r[0]] = r
            b.writers = list(last.values())
        return ins

    def finish(self, bufs):
        E = self.E["sp"]
        for b in bufs:
            for ev in b.writers + b.readers:
                self._wait_ev(E, ev)


def hi(ap):
    v = ap.bitcast(BF16)
    n = len(v.shape)
    names = "abcdefg"[: n - 1]
    src = " ".join(names) + " (n t)"
    dst = " ".join(names) + " n t"
    v = v.rearrange(f"{src} -> {dst}", t=2)
    idx = tuple([slice(None)] * n + [1])
    return v[idx]


class Ctx:
    def __init__(self, nc, P):
        self.nc = nc
        self.P = P
        self.ps_all = nc.alloc_psum_tensor("ps_all", [128, 4096], F32)
        self.pb = [self.ps_all[:, 512 * i:512 * (i + 1)] for i in range(8)]
        self.Bpb = [P.buf(f"pb{i}") for i in range(8)]
        self.ones_f = nc.alloc_sbuf_tensor("s_ones_f", [128, 128], F32)
        self.ident_f = nc.alloc_sbuf_tensor("s_ident_f", [128, 128], F32)
        self.ident_b = nc.alloc_sbuf_tensor("s_ident_b", [128, 128], BF16)
        self.maskT_f = nc.alloc_sbuf_tensor("s_maskT_f", [128, 128], F32)
        self.maskT_b = nc.alloc_sbuf_tensor("s_maskT_b", [128, 128], BF16)
        self.ones_b = nc.alloc_sbuf_tensor("s_ones_b", [128, 128], BF16)
        self.Bconst = P.buf("const")
        B = self.Bconst
        P.op("pool", lambda e: e.memset(self.ones_f[:], 1.0), writes=[B])
        P.op("pool", lambda e: e.memset(self.ones_b[:], 1.0), writes=[B], accumulate=True)
        P.op("pool", lambda e: e.affine_select(out=self.ident_f[:], in_=self.ones_f[:],
                                               pattern=[[-1, 128]], compare_op=ALU.is_equal,
                                               fill=0.0, base=0, channel_multiplier=1),
             reads=[B], writes=[B], accumulate=True)
        P.op("pool", lambda e: e.tensor_copy(out=self.ident_b[:], in_=self.ident_f[:]),
             reads=[B], writes=[B], accumulate=True)
        P.op("pool", lambda e: e.memset(self.maskT_f[:], -1.0e5), writes=[B], accumulate=True)
        P.op("pool", lambda e: e.affine_select(out=self.maskT_f[:], in_=self.maskT_f[:],
                                               pattern=[[-1, 128]], compare_op=ALU.is_gt,
                                               fill=0.0, base=0, channel_multiplier=1),
             reads=[B], writes=[B], accumulate=True)
        P.op("pool", lambda e: e.tensor_copy(out=self.maskT_b[:], in_=self.maskT_f[:]),
             reads=[B], writes=[B], accumulate=True)


def attn_core(C, items, out_cb):
    P, nc = C.P, C.nc
    PT = C.PT
    BPT = C.BPT
    work = []
    for it in items:
        for I in range(16):
            nk = it["nkb"](I)
            for J in range(nk):
                work.append((it, I, J, nk))
    sidx = 0
    oidx = 0
    state = {}

    def emit_S(w, k):
        it, I, J, nk = w
        bank = k % 3
        diag = it["causal"] and J >= 4 * I
        c0 = 128 * (J - 4 * I) if diag else 0
        K = it["K"]
        ps = C.pb[bank]
        P.op("pe", lambda e: e.matmul(ps[:, c0:512], it["KT"](J), it["QT"](I)[:, c0:512],
                                      start=True, stop=not diag),
             reads=it["reads"], writes=[C.Bpb[bank]])
        if diag:
            P.op("pe", lambda e: e.matmul(ps[:, c0:c0 + 128], C.ident_b[:], C.maskT_b[:],
                                          start=False, stop=True),
                 reads=[C.Bconst], writes=[C.Bpb[bank]], accumulate=True)
        return c0

    def emit_exp(w, k, c0):
        it, I, J, nk = w
        bank = k % 3
        pt = k % len(PT)
        b = it["bias"](I, J) if it["bias"] is not None else 0.0
        rd = [C.Bpb[bank]] + (it["bias_reads"] if it["bias"] is not None else [])
        P.op("act", lambda e: e.activation(out=PT[pt][:, c0:512], in_=C.pb[bank][:, c0:512],
                                           func=AF.Exp, bias=b, scale=it["scale"]),
             reads=rd, writes=[BPT[pt]])

    def emit_PV(w, k, c0, ob):
        it, I, J, nk = w
        pt = k % len(PT)
        po = C.pb[3 + ob]
        P.op("pe", lambda e: e.matmul(po[:, c0:512], it["VA"](J), PT[pt][:, c0:512],
                                      start=(J == 0), stop=(J == nk - 1)),
             reads=[BPT[pt]] + it["reads"], writes=[C.Bpb[3 + ob]], accumulate=(J != 0))

    def emit_norm(w, ob, gi):
        it, I, J, nk = w
        po = C.pb[3 + ob]
        st = gi % 2
        P.op("dve", lambda e: e.reciprocal(out=C.rinv[64:128, :], in_=po[64:128, :]),
             reads=[C.Bpb[3 + ob]], writes=[C.Brinv])
        P.op("dve", lambda e: e.tensor_tensor(out=C.OTs[st][0:64, :], in0=po[0:64, :],
                                              in1=C.rinv[64:128, :], op=ALU.mult),
             reads=[C.Bpb[3 + ob], C.Brinv], writes=[C.BOTs[st]])
        out_cb(it, I, C.OTs[st], C.BOTs[st])

    n = len(work)
    LOOK = 2
    c0s = {}
    gi = 0
    obs = {}
    gcount = 0
    for k in range(min(LOOK, n)):
        c0s[k] = emit_S(work[k], k)
    for k in range(n):
        w = work[k]
        it, I, J, nk = w
        if J == 0:
            obs[(id(it), I)] = gcount % 2
            gcount += 1
        ob = obs[(id(it), I)]
        emit_exp(w, k, c0s[k])
        if k + LOOK < n:
            c0s[k + LOOK] = emit_S(work[k + LOOK], k + LOOK)
        emit_PV(w, k, c0s[k], ob)
        if J == nk - 1:
            emit_norm(w, ob, gi)
            gi += 1


def alloc_attn_common(C):
    nc, P = C.nc, C.P
    C.PT = [nc.alloc_sbuf_tensor(f"s_PT{i}", [128, 512], BF16) for i in range(4)]
    C.BPT = [P.buf(f"PT{i}") for i in range(4)]
    C.rinv = nc.alloc_sbuf_tensor("s_rinv", [128, 512], F32)
    C.Brinv = P.buf("rinv")
    C.OTs = [nc.alloc_sbuf_tensor(f"s_OTs{i}", [64, 512], BF16) for i in range(2)]
    C.BOTs = [P.buf(f"OTs{i}") for i in range(2)]


def phase_fox(C, io):
    nc, P = C.nc, C.P
    xT, OT = io["xT"], io["OT"]
    NB = S // 512
    alloc_attn_common(C)
    wq = nc.alloc_sbuf_tensor("s_wq", [128, 8, 192], F32)
    wk = nc.alloc_sbuf_tensor("s_wk", [128, 8, 192], F32)
    wvf = nc.alloc_sbuf_tensor("s_wvf", [128, 8, 3, 66], F32)
    wmq = nc.alloc_sbuf_tensor("s_wmq", [128, 8, 64], F32)
    wmk = nc.alloc_sbuf_tensor("s_wmk", [128, 8, 64], F32)
    wmv = nc.alloc_sbuf_tensor("s_wmv", [128, 8, 64], F32)
    memf = nc.alloc_sbuf_tensor("s_memf", [128, 8, 256], F32)
    memb = nc.alloc_sbuf_tensor("s_memb", [128, 8, 256], BF16)
    nbf = nc.alloc_sbuf_tensor("s_nbf", [128, 4], F32)
    Bw = P.buf("w")
    r = lambda ap: ap.rearrange("(c p) n -> p c n", p=128)
    P.dma("sp", wq[:], r(io["wq"]), Bw, writes=[Bw], group="w", accumulate=True)
    Bw2 = P.buf("w2")
    P.dma("sp", wk[:], r(io["wk"]), Bw2, writes=[Bw2], group="w", accumulate=True)
    Bw3 = P.buf("w3")
    P.op("pool", lambda e: e.memset(wvf[:], 0.0), writes=[Bw3])
    for h in range(3):
        P.dma("sp", wvf[:, :, h, 0:64], r(io["wv"])[:, :, 64 * h:64 * h + 64], Bw3,
              writes=[Bw3], group="w", accumulate=True)
    with nc.allow_non_contiguous_dma(reason="tiny gate columns"):
        for h in range(3):
            P.dma("sp", wvf[:, :, h, 64:65], r(io["wf"])[:, :, h:h + 1], Bw3,
                  writes=[Bw3], group="w", accumulate=True)
    Bw4 = P.buf("w4")
    P.dma("sp", wmq[:], r(io["wmq"]), Bw4, writes=[Bw4], group="w", accumulate=True)
    P.dma("sp", wmk[:], r(io["wmk"]), Bw4, writes=[Bw4], group="w", accumulate=True)
    P.dma("sp", wmv[:], r(io["wmv"]), Bw4, writes=[Bw4], group="w", accumulate=True)
    Bmem = P.buf("mem")
    P.dma("sp", memf[:], r(io["memT"]), Bmem, writes=[Bmem])
    Bnbf = P.buf("nbf")
    P.dma("sp", nbf[:, 0:3], io["nbf"], Bnbf, writes=[Bnbf])
    Bmemb = P.buf("memb")
    P.op("pool", lambda e: e.tensor_copy(out=memb[:], in_=memf[:]), reads=[Bmem], writes=[Bmemb])

    QT = nc.alloc_sbuf_tensor("s_QT", [65, S], BF16)
    KT = nc.alloc_sbuf_tensor("s_KT", [65, S], BF16)
    VA = nc.alloc_sbuf_tensor("s_VA", [128, 64, 128], BF16)
    MQT = nc.alloc_sbuf_tensor("s_MQT", [64, S], BF16)
    MKT = nc.alloc_sbuf_tensor("s_MKT", [64, 256], BF16)
    MVA = nc.alloc_sbuf_tensor("s_MVA", [128, 2, 128], BF16)
    BQT, BKT, BVA, BMQT, BMK, BMV = [P.buf(n) for n in ("QT", "KT", "VA", "MQT", "MKT", "MVA")]
    xs_f = [nc.alloc_sbuf_tensor(f"s_xsf{i}", [128, 8, 512], F32) for i in range(2)]
    xs_b = [nc.alloc_sbuf_tensor(f"s_xsb{i}", [128, 8, 512], BF16) for i in range(2)]
    Bxf = [P.buf(f"xsf{i}") for i in range(2)]
    Bxb = [P.buf(f"xsb{i}") for i in range(2)]
    fTok = nc.alloc_sbuf_tensor("s_fTok", [128, 64], F32)
    Bf = P.buf("fTok")
    lf = nc.alloc_sbuf_tensor("s_lf", [128, 64], F32)
    zero64 = nc.alloc_sbuf_tensor("s_zero64", [64, 128], F32)
    within = nc.alloc_sbuf_tensor("s_within", [64, 128], F32)
    cumb = nc.alloc_sbuf_tensor("s_cumb", [64, 128], F32)
    augb = nc.alloc_sbuf_tensor("s_augb", [64, 128], BF16)
    su64 = nc.alloc_sbuf_tensor("s_su64", [64, 64], F32)
    g64 = nc.alloc_sbuf_tensor("s_g64", [64, 16, 4], F32)
    offd = nc.alloc_sbuf_tensor("s_offd", [64, 2], F32)
    offrow = nc.alloc_sbuf_tensor("s_offrow", [1, 64], F32)
    cqB = nc.alloc_sbuf_tensor("s_cqB", [128, 16], F32)
    cumTok = nc.alloc_sbuf_tensor("s_cumTok", [128, 64], F32)
    biasT = nc.alloc_sbuf_tensor("s_biasT", [128, 16, 64], F32)
    Bcum = P.buf("cum")
    Bbias = P.buf("bias")
    Baug = P.buf("aug")
    Bscr = P.buf("augscr")
    Bk = C.Bconst
    P.op("pool", lambda e: e.memset(zero64[:], 0.0), writes=[Bk], accumulate=True)
    P.op("pool", lambda e: e.affine_select(out=su64[:], in_=C.ones_f[0:64, 0:64], pattern=[[1, 64]],
                                           compare_op=ALU.is_gt, fill=0.0, base=0,
                                           channel_multiplier=-1),
         reads=[Bk], writes=[Bk], accumulate=True)
    P.op("pool", lambda e: e.affine_select(out=g64[:].rearrange("p a b -> p (a b)"), in_=su64[:],
                                           pattern=[[-4, 16], [0, 4]], compare_op=ALU.is_ge,
                                           fill=0.0, base=0, channel_multiplier=1),
         reads=[Bk], writes=[Bk], accumulate=True)
    P.op("pool", lambda e: e.memset(VA[:, :, 64:128], 1.0), writes=[BVA])
    P.op("pool", lambda e: e.memset(MVA[:, :, 64:128], 1.0), writes=[BMV])
    P.op("pool", lambda e: e.memset(KT[64:65, :], 1.0), writes=[BKT])

    for c in range(8):
        P.op("pe", lambda e: e.matmul(C.pb[5][0:64, 0:256], hi(wmk[:, c, :]), memb[:, c, :],
                                      start=(c == 0), stop=(c == 7)),
             reads=[Bw4, Bmemb], writes=[C.Bpb[5]], accumulate=(c != 0))
    P.op("act", lambda e: e.copy(out=MKT[:], in_=C.pb[5][0:64, 0:256]), reads=[C.Bpb[5]], writes=[BMK])
    for mb in range(2):
        for c in range(8):
            P.op("pe", lambda e: e.matmul(C.pb[6][:, 64 * mb:64 * mb + 64],
                                          memb[:, c, 128 * mb:128 * mb + 128], hi(wmv[:, c, :]),
                                          start=(c == 0), stop=(c == 7)),
                 reads=[Bw4, Bmemb], writes=[C.Bpb[6]], accumulate=not (c == 0 and mb == 0))
    P.op("act", lambda e: e.copy(out=MVA[:, :, 0:64],
                                 in_=C.pb[6][:, 0:128].rearrange("p (a b) -> p a b", a=2)),
         reads=[C.Bpb[6]], writes=[BMV], accumulate=True)

    xTr = r(xT)
    seq = [0]

    def load_x(nb):
        i = seq[0] % 2
        seq[0] += 1
        P.dma("sp", xs_f[i][:], xTr[:, :, nb * 512:(nb + 1) * 512], Bxf[i], writes=[Bxf[i]])
        P.op("pool", lambda e: e.tensor_copy(out=xs_b[i][:], in_=xs_f[i][:]),
             reads=[Bxf[i]], writes=[Bxb[i]])
        return i

    def out_cb_factory(row0):
        def cb(it, I, st_tile, st_buf):
            P.dma("sp", OT[row0:row0 + 64, I * 512:(I + 1) * 512], st_tile[0:64, :], st_buf,
                  reads=[st_buf], writes=[io["BOT"]], accumulate=True)
        return cb

    for h in range(3):
        nxt = load_x(0)
        for nb in range(NB):
            i = nxt
            if nb + 1 < NB:
                nxt = load_x(nb + 1)
            par = nb % 2
            bq, bk, bv = 5 + 0, 6, 7
            for (wt, bank, dst, Bdst, Bwt) in ((wq, 5, QT, BQT, Bw), (wk, 6, KT, BKT, Bw2)):
                for c in range(8):
                    P.op("pe", lambda e: e.matmul(C.pb[bank][0:64, :], hi(wt[:, c, 64 * h:64 * h + 64]),
                                                  xs_b[i][:, c, :], start=(c == 0), stop=(c == 7)),
                         reads=[Bwt, Bxb[i]], writes=[C.Bpb[bank]], accumulate=(c != 0))
                P.op("act", lambda e: e.copy(out=dst[0:64, nb * 512:(nb + 1) * 512],
                                             in_=C.pb[bank][0:64, :]),
                     reads=[C.Bpb[bank]], writes=[Bdst], accumulate=True)
            if h == 0:
                for c in range(8):
                    P.op("pe", lambda e: e.matmul(C.pb[5][0:64, :], hi(wmq[:, c, :]), xs_b[i][:, c, :],
                                                  start=(c == 0), stop=(c == 7)),
                         reads=[Bw4, Bxb[i]], writes=[C.Bpb[5]], accumulate=(c != 0))
                P.op("act", lambda e: e.copy(out=MQT[0:64, nb * 512:(nb + 1) * 512],
                                             in_=C.pb[5][0:64, :]),
                     reads=[C.Bpb[5]], writes=[BMQT], accumulate=True)
            for sb in range(4):
                for c in range(8):
                    P.op("pe", lambda e: e.matmul(C.pb[7][:, 128 * sb:128 * sb + 65],
                                                  xs_b[i][:, c, 128 * sb:128 * sb + 128],
                                                  hi(wvf[:, c, h, :])[:, 0:65],
                                                  start=(c == 0), stop=(c == 7)),
                         reads=[Bw3, Bxb[i]], writes=[C.Bpb[7]],
                         accumulate=not (c == 0 and sb == 0))
            pv = C.pb[7][:].rearrange("p (a b) -> p a b", a=4)
            P.op("dve", lambda e: e.tensor_copy(out=VA[:, 4 * nb:4 * nb + 4, 0:64], in_=pv[:, :, 0:64]),
                 reads=[C.Bpb[7]], writes=[BVA], accumulate=True)
            P.op("dve", lambda e: e.tensor_copy(out=fTok[:, 4 * nb:4 * nb + 4], in_=pv[:, :, 64]),
                 reads=[C.Bpb[7]], writes=[Bf], accumulate=True)

        P.op("dve", lambda e: e.tensor_scalar(out=lf[:], in0=fTok[:], scalar1=nbf[:, h:h + 1],
                                              scalar2=None, op0=ALU.add),
             reads=[Bf, Bnbf], writes=[Bcum])
        P.op("act", lambda e: e.activation(out=lf[:], in_=lf[:], func=AF.Exp, scale=-1.0),
             reads=[Bcum], writes=[Bcum])
        P.op("act", lambda e: e.activation(out=lf[:], in_=lf[:], func=AF.Ln, bias=1.0, scale=1.0),
             reads=[Bcum], writes=[Bcum])
        P.op("pe", lambda e: e.transpose(C.pb[5][0:64, 0:128], lf[:], C.ident_f[:]),
             reads=[Bcum, C.Bconst], writes=[C.Bpb[5]])
        P.op("dve", lambda e: e.tensor_tensor_scan(out=within[:], data0=zero64[:],
                                                   data1=C.pb[5][0:64, 0:128], initial=0.0,
                                                   op0=ALU.add, op1=ALU.subtract),
             reads=[C.Bpb[5], C.Bconst], writes=[Bcum], accumulate=True)
        P.op("pe", lambda e: e.matmul(C.pb[6][0:64, 0:1], su64[:], within[:, 127:128],
                                      start=True, stop=True),
             reads=[Bcum, C.Bconst], writes=[C.Bpb[6]])
        P.op("pe", lambda e: e.matmul(C.pb[6][0:64, 2:3], g64[:].rearrange("p a b -> p (a b)"),
                                      within[:, 127:128], start=True, stop=True),
             reads=[Bcum, C.Bconst], writes=[C.Bpb[6]], accumulate=True)
        P.op("dve", lambda e: e.tensor_copy(out=offd[:], in_=C.pb[6][0:64, 0:4].rearrange(
            "p (a b) -> p a b", b=2)[:, :, 0]), reads=[C.Bpb[6]], writes=[Bcum], accumulate=True)
        P.op("dve", lambda e: e.tensor_scalar(out=cumb[:], in0=within[:], scalar1=offd[:, 0:1],
                                              scalar2=None, op0=ALU.add),
             reads=[Bcum], writes=[Bcum], accumulate=True)
        P.op("dve", lambda e: e.tensor_scalar(out=augb[:], in0=within[:], scalar1=offd[:, 1:2],
                                              scalar2=8.0, op0=ALU.add, op1=ALU.mult),
             reads=[Bcum], writes=[Bcum], accumulate=True)
        P.dma("sp", io["augscr"], augb[:], Baug, reads=[Bcum], writes=[Bscr])
        P.dma("sp", QT[64:65, :], io["augscr"].rearrange("(o b) t -> o (b t)", o=1), Baug,
              reads=[Bscr], writes=[BQT], accumulate=True)
        P.op("pe", lambda e: e.transpose(C.pb[5][:, 128:192], cumb[:], C.ident_f[0:64, 0:64]),
             reads=[Bcum, C.Bconst], writes=[C.Bpb[5]])
        P.op("dve", lambda e: e.tensor_copy(out=cumTok[:], in_=C.pb[5][:, 128:192]),
             reads=[C.Bpb[5]], writes=[Bbias])
        P.op("pe", lambda e: e.transpose(C.pb[6][0:1, 64:128], offd[:, 0:1], C.ident_f[0:64, 0:64]),
             reads=[Bcum, C.Bconst], writes=[C.Bpb[6]])
        P.op("dve", lambda e: e.tensor_copy(out=offrow[:], in_=C.pb[6][0:1, 64:128]),
             reads=[C.Bpb[6]], writes=[Bbias], accumulate=True)
        P.op("pe", lambda e: e.matmul(C.pb[6][:, 128:144], C.ones_f[0:1, :],
                                      offrow[:].rearrange("o (a b) -> o a b", b=4)[:, :, 0],
                                      start=True, stop=True),
             reads=[Bbias, C.Bconst], writes=[C.Bpb[6]])
        P.op("dve", lambda e: e.tensor_copy(out=cqB[:], in_=C.pb[6][:, 128:144]),
             reads=[C.Bpb[6]], writes=[Bbias], accumulate=True)
        for I in range(16):
            nk = 4 * I + 4
            P.op("dve", lambda e: e.tensor_scalar(out=biasT[:, I, 0:nk], in0=cumTok[:, 0:nk],
                                                  scalar1=-1.0, scalar2=cqB[:, I:I + 1],
                                                  op0=ALU.mult, op1=ALU.add),
                 reads=[Bbias], writes=[Bbias], accumulate=True)

        items = [dict(K=65, QT=lambda I: QT[0:65, I * 512:(I + 1) * 512],
                      KT=lambda J: KT[0:65, J * 128:(J + 1) * 128],
                      VA=lambda J: VA[:, J, :], nkb=lambda I: 4 * I + 4, causal=True,
                      scale=0.125, bias=lambda I, J: biasT[:, I, J:J + 1],
                      bias_reads=[Bbias], reads=[BQT, BKT, BVA], row0=64 * h)]
        if h == 0:
            items.append(dict(K=64, QT=lambda I: MQT[0:64, I * 512:(I + 1) * 512],
                              KT=lambda J: MKT[0:64, J * 128:(J + 1) * 128],
                              VA=lambda J: MVA[:, J, :], nkb=lambda I: 2, causal=False,
                              scale=0.125, bias=None, reads=[BMQT, BMK, BMV], row0=192))

        def cb(it, I, st_tile, st_buf):
            r0 = it["row0"]
            P.dma("sp", OT[r0:r0 + 64, I * 512:(I + 1) * 512], st_tile[0:64, :], st_buf,
                  reads=[st_buf], writes=[io["BOT"]], accumulate=True)
        attn_core(C, items, cb)


def build_fox():
    nc = bass.Bass("TRN2", target_bir_lowering=False)
    P = Prog(nc)
    io = {}
    io["xT"] = nc.dram_tensor("xT", [D, S], F32, kind="ExternalInput").ap()
    for n, w in (("wq", 192), ("wk", 192), ("wv", 192), ("wf", 3), ("wmq", 64), ("wmk", 64), ("wmv", 64)):
        io[n] = nc.dram_tensor(n, [D, w], F32, kind="ExternalInput").ap()
    io["memT"] = nc.dram_tensor("memT", [D, 256], F32, kind="ExternalInput").ap()
    io["nbf"] = nc.dram_tensor("nbf", [128, 3], F32, kind="ExternalInput").ap()
    io["OT"] = nc.dram_tensor("OT", [256, S], BF16, kind="ExternalOutput").ap()
    io["augscr"] = nc.dram_tensor("augscr", [64, 128], BF16, kind="Internal").ap()
    io["BOT"] = P.buf("OT")
    C = Ctx(nc, P)
    phase_fox(C, io)
    P.finish([io["BOT"]])
    return nc


CAP = 384
NE = 32
NT = 16
ALPHA = (2 * 2) ** 0.25
LN_EPS = 1e-5


def layer_norm_tile(C, y, By, g_rep, b_rep, Bgb, out, Bout, sc):
    P = C.P
    st, mv, sd, Bs = sc["st"], sc["mv"], sc["sd"], sc["B"]
    P.op("dve", lambda e: e.bn_stats(out=st[:, 0, :], in_=y[:, 0:512]), reads=[By], writes=[Bs])
    P.op("dve", lambda e: e.bn_stats(out=st[:, 1, :], in_=y[:, 512:1024]), reads=[By], writes=[Bs],
         accumulate=True)
    P.op("dve", lambda e: e.bn_aggr(out=mv[:], in_=st[:].rearrange("p a b -> p (a b)")),
         reads=[Bs], writes=[Bs], accumulate=True)
    P.op("act", lambda e: e.activation(out=sd[:, 0:1], in_=mv[:, 1:2], func=AF.Sqrt,
                                       bias=C.eps_t[:, 0:1], scale=1.0),
         reads=[Bs, C.Bconst], writes=[Bs], accumulate=True)
    P.op("dve", lambda e: e.reciprocal(out=sd[:, 1:2], in_=sd[:, 0:1]), reads=[Bs], writes=[Bs],
         accumulate=True)
    P.op("dve", lambda e: e.tensor_scalar(out=out[:], in0=y[:], scalar1=mv[:, 0:1],
                                          scalar2=sd[:, 1:2], op0=ALU.subtract, op1=ALU.mult),
         reads=[By, Bs], writes=[Bout])
    P.op("pool", lambda e: e.tensor_tensor(out=out[:], in0=out[:], in1=g_rep, op=ALU.mult),
         reads=[Bout, Bgb], writes=[Bout])
    P.op("pool", lambda e: e.tensor_tensor(out=out[:], in0=out[:], in1=b_rep, op=ALU.add),
         reads=[Bout, Bgb], writes=[Bout])


def phase_token(C, io):
    from contextlib import ExitStack
    nc, P = C.nc, C.P
    xres, OTall, xo = io["xres"], io["OTall"], io["xo"]
    xe_d, ye_d, x1f_d = io["xe"], io["ye"], io["x1f"]
    Bxe, Bye, Bx1f, Bxo = P.buf("xe"), P.buf("ye"), P.buf("x1f"), io["Bxo"]
    rr = lambda ap: ap.rearrange("(c p) n -> p c n", p=128)

    C.eps_t = nc.alloc_sbuf_tensor("s_eps", [128, 1], F32)
    P.op("pool", lambda e: e.memset(C.eps_t[:], LN_EPS), writes=[C.Bconst], accumulate=True)
    gur = [nc.alloc_sbuf_tensor(f"s_gur{i}", [128, 8, 2, 256], F32) for i in range(4)]
    dnr = [nc.alloc_sbuf_tensor(f"s_dnr{i}", [128, 8, 512], F32) for i in range(2)]
    Bgur = [P.buf(f"gur{i}") for i in range(4)]
    Bdnr = [P.buf(f"dnr{i}") for i in range(2)]
    lngb = nc.alloc_sbuf_tensor("s_lngb", [128, 4, 1024], F32)
    Blngb = P.buf("lngb")
    P.dma("sp", lngb[:], io["lngb"], Blngb, writes=[Blngb])
    g4all = nc.alloc_sbuf_tensor("s_g4all", [128, NT, 4], F32)
    idxall = nc.alloc_sbuf_tensor("s_idxall", [128, NT, 4], I32)
    GT = nc.alloc_sbuf_tensor("s_GT", [32, NT, 128], F32)
    bdn = nc.alloc_sbuf_tensor("s_bdn", [32, 1024], F32)
    Bg4, Bidx, BGT, Bbdn = P.buf("g4"), P.buf("idx"), P.buf("GT"), P.buf("bdn")
    P.dma("sp", bdn[:], io["b_dn"], Bbdn, writes=[Bbdn])
    bgu = nc.alloc_sbuf_tensor("s_bgu", [128, NE * 16], F32)
    Bbgu = P.buf("bgu")

    w_gu = io["w_gu"].rearrange("e (c p) (two f) -> e p c two f", p=128, two=2)
    w_dn = io["w_dn"].rearrange("e (c p) m -> e p c m", p=128)
    gu_n = [0]
    dn_n = [0]

    def load_gu():
        n = gu_n[0]
        gu_n[0] += 1
        e, q = n // 4, n % 4
        if e >= NE:
            return
        s = n % 4
        for two in range(2):
            P.dma("sp", gur[s][:, :, two, :], w_gu[e][:, :, two, 256 * q:256 * q + 256], Bgur[s],
                  writes=[Bgur[s]], group=("gu", n), accumulate=(two != 0))

    def load_dn():
        n = dn_n[0]
        dn_n[0] += 1
        e, hf = n // 2, n % 2
        if e >= NE:
            return
        s = n % 2
        P.dma("sp", dnr[s][:], w_dn[e][:, :, 512 * hf:512 * hf + 512], Bdnr[s], writes=[Bdnr[s]])

    with ExitStack() as es:
        T = lambda name, shape, dt: es.enter_context(nc.sbuf_tensor("s_" + name, shape, dt))
        wout = T("wout", [128, 8, 1024], F32)
        wr = T("wr", [128, 8, 32], F32)
        brr = T("brr", [128, 32], F32)
        eoff = T("eoff", [128, 32], F32)
        ustr_f = T("ustr_f", [128, 128], F32)
        ustr = T("ustr", [128, 128], BF16)
        carry = T("carry", [128, 32], F32)
        bgu_raw = T("bgu_raw", [128, 4, 128], F32)
        ots = [T(f"ot{i}", [128, 8, 128], BF16) for i in range(2)]
        xts = [T(f"xt{i}", [128, 1024], F32) for i in range(2)]
        yt = T("yt", [128, 1024], F32)
        x1s = [T(f"x1_{i}", [128, 1024], F32) for i in range(2)]
        x1bs = [T(f"x1b{i}", [128, 1024], BF16) for i in range(2)]
        x1T = T("x1T", [128, 8, 128], F32)
        lg = T("lg", [128, 32], F32)
        m8 = T("m8", [128, 8], F32)
        maskb = T("maskb", [128, 32], BF16)
        posf = T("posf", [128, 32], F32)
        sbf = T("sbf", [128, 32], F32)
        oh = T("oh", [128, 32], F32)
        junk = T("junk", [128, 32], F32)
        Gd = T("Gd", [128, 32], F32)
        e4 = T("e4", [128, 4], F32)
        sm = T("sm", [128, 4], F32)
        slotf = T("slotf", [128, 4], F32)
        lnsc = dict(st=T("lnst", [128, 2, 6], F32), mv=T("lnmv", [128, 2], F32),
                    sd=T("lnsd", [128, 2], F32), B=P.buf("lnsc"))
        Bwout, Bwr, Bmisc = P.buf("wout"), P.buf("wr"), P.buf("misc")
        Bots = [P.buf(f"ot{i}") for i in range(2)]
        Bxts = [P.buf(f"xt{i}") for i in range(2)]
        Byt = P.buf("yt")
        Bx1s = [P.buf(f"x1_{i}") for i in range(2)]
        Bx1bs = [P.buf(f"x1b{i}") for i in range(2)]
        Bx1T, Brt, Bcarry = P.buf("x1T"), P.buf("rt"), P.buf("carry")

        P.dma("sp", wout[:], rr(io["wout"]), Bwout, writes=[Bwout])
        P.dma("sp", wr[:], rr(io["w_r"]), Bwr, writes=[Bwr])
        P.dma("sp", brr[:], io["b_r"], Bwr, writes=[Bwr], accumulate=True, group="wr")
        P.dma("sp", bgu_raw[:], io["b_gu"].rearrange("e (c f) -> (e c) f", f=128).rearrange(
            "(a p) f -> p a f", p=128), Bbgu, writes=[Bbgu])
        for a in range(4):
            P.op("pe", lambda e: e.transpose(C.pb[0][:, 128 * a:128 * a + 128], bgu_raw[:, a, :],
                                             C.ident_f[:]),
                 reads=[Bbgu, C.Bconst], writes=[C.Bpb[0]], accumulate=(a != 0))
        P.op("dve", lambda e: e.tensor_copy(out=bgu[:], in_=C.pb[0]), reads=[C.Bpb[0]], writes=[Bbgu])
        bgu3 = bgu[:].rearrange("p (e c) -> p e c", c=16)
        P.op("dve", lambda e: e.tensor_scalar(out=bgu3[:, :, 8:16], in0=bgu3[:, :, 8:16], scalar1=1.0,
                                              scalar2=None, op0=ALU.add),
             reads=[Bbgu], writes=[Bbgu])
        P.op("pool", lambda e: e.iota(eoff[:], pattern=[[CAP, 32]], base=0, channel_multiplier=0,
                                      allow_small_or_imprecise_dtypes=True), writes=[Bmisc])
        P.op("pool", lambda e: e.affine_select(out=ustr_f[:], in_=C.ones_f[:], pattern=[[1, 128]],
                                               compare_op=ALU.is_gt, fill=0.0, base=0,
                                               channel_multiplier=-1),
             reads=[C.Bconst], writes=[Bmisc], accumulate=True)
        P.op("pool", lambda e: e.tensor_copy(out=ustr[:], in_=ustr_f[:]), reads=[Bmisc], writes=[Bmisc],
             accumulate=True)
        P.op("pool", lambda e: e.memset(carry[:], 0.0), writes=[Bcarry])
        for _ in range(4):
            load_gu()
        for _ in range(2):
            load_dn()

        OTr = rr(OTall)

        def load_tile(i):
            P.dma("sp", ots[i % 2][:], OTr[:, :, 128 * i:128 * i + 128], Bots[i % 2], writes=[Bots[i % 2]])
            P.dma("sp", xts[i % 2][:], xres[128 * i:128 * i + 128, :], Bxts[i % 2], writes=[Bxts[i % 2]])

        load_tile(0)
        for i in range(NT):
            if i + 1 < NT:
                load_tile(i + 1)
            ot, xt = ots[i % 2], xts[i % 2]
            x1, x1b = x1s[i % 2], x1bs[i % 2]
            Bx1, Bx1b = Bx1s[i % 2], Bx1bs[i % 2]
            for hf in range(2):
                for c in range(8):
                    P.op("pe", lambda e: e.matmul(C.pb[hf], ot[:, c, :], hi(wout[:, c, 512 * hf:512 * hf + 512]),
                                                  start=(c == 0), stop=(c == 7)),
                         reads=[Bots[i % 2], Bwout], writes=[C.Bpb[hf]], accumulate=(c != 0))
                P.op("dve", lambda e: e.scalar_tensor_tensor(out=yt[:, 512 * hf:512 * hf + 512],
                                                             in0=xt[:, 512 * hf:512 * hf + 512],
                                                             scalar=ALPHA, in1=C.pb[hf],
                                                             op0=ALU.mult, op1=ALU.add),
                     reads=[Bxts[i % 2], C.Bpb[hf]], writes=[Byt], accumulate=(hf != 0))
            layer_norm_tile(C, yt, Byt, lngb[:, 0, :], lngb[:, 1, :], Blngb, x1, Bx1, lnsc)
            P.dma("sp", x1f_d[128 * i:128 * i + 128, :], x1[:], Bx1, reads=[Bx1], writes=[Bx1f],
                  accumulate=True)
            P.op("act", lambda e: e.copy(out=x1b[:], in_=x1[:]), reads=[Bx1], writes=[Bx1b])
            for half in range(2):
                for c4 in range(4):
                    c = 4 * half + c4
                    P.op("pe", lambda e: e.transpose(C.pb[2 + half][:, 128 * c4:128 * c4 + 128],
                                                     x1[:, 128 * c:128 * c + 128], C.ident_f[:]),
                         reads=[Bx1, C.Bconst], writes=[C.Bpb[2 + half]], accumulate=(c4 != 0))
                P.op("act", lambda e: e.copy(out=x1T[:, 4 * half:4 * half + 4, :].rearrange("p a b -> p (a b)"),
                                             in_=C.pb[2 + half]),
                     reads=[C.Bpb[2 + half]], writes=[Bx1T], accumulate=(half != 0))
            for c in range(8):
                P.op("pe", lambda e: e.matmul(C.pb[4][:, 0:32], x1T[:, c, :], wr[:, c, :],
                                              start=(c == 0), stop=(c == 7)),
                     reads=[Bx1T, Bwr], writes=[C.Bpb[4]], accumulate=(c != 0))
            P.op("dve", lambda e: e.tensor_tensor(out=lg[:], in0=C.pb[4][:, 0:32], in1=brr[:], op=ALU.add),
                 reads=[C.Bpb[4], Bwr], writes=[Brt])
            P.op("dve", lambda e: e.max(out=m8[:], in_=lg[:]), reads=[Brt], writes=[Brt], accumulate=True)
            P.op("dve", lambda e: e.tensor_scalar(out=maskb[:], in0=lg[:], scalar1=m8[:, 3:4], scalar2=None,
                                                  op0=ALU.is_ge),
                 reads=[Brt], writes=[Brt], accumulate=True)
            P.op("pe", lambda e: e.matmul(C.pb[5][:, 0:32], ustr[:], maskb[:], start=True, stop=True),
                 reads=[Brt, Bmisc], writes=[C.Bpb[5]])
            P.op("pe", lambda e: e.matmul(C.pb[5][:, 32:64], C.ones_b[:], maskb[:], start=True, stop=True),
                 reads=[Brt, C.Bconst], writes=[C.Bpb[5]], accumulate=True)
            P.op("dve", lambda e: e.tensor_tensor(out=posf[:], in0=C.pb[5][:, 0:32], in1=carry[:], op=ALU.add),
                 reads=[C.Bpb[5], Bcarry], writes=[Brt], accumulate=True)
            P.op("dve", lambda e: e.tensor_tensor(out=carry[:], in0=C.pb[5][:, 32:64], in1=carry[:], op=ALU.add),
                 reads=[C.Bpb[5], Bcarry], writes=[Bcarry])
            P.op("dve", lambda e: e.scalar_tensor_tensor(out=sbf[:], in0=posf[:], scalar=float(CAP - 1),
                                                         in1=eoff[:], op0=ALU.min, op1=ALU.add),
                 reads=[Brt, Bmisc], writes=[Brt], accumulate=True)
            P.op("dve", lambda e: e.tensor_scalar(out=sm[:, 0:1], in0=m8[:, 0:1], scalar1=-1.0, scalar2=None,
                                                  op0=ALU.mult),
                 reads=[Brt], writes=[Brt], accumulate=True)
            P.op("act", lambda e: e.activation(out=e4[:], in_=m8[:, 0:4], func=AF.Exp, bias=sm[:, 0:1],
                                               scale=1.0, accum_out=sm[:, 1:2]),
                 reads=[Brt], writes=[Brt], accumulate=True)
            P.op("dve", lambda e: e.reciprocal(out=sm[:, 2:3], in_=sm[:, 1:2]), reads=[Brt], writes=[Brt],
                 accumulate=True)
            P.op("dve", lambda e: e.tensor_scalar(out=g4all[:, i, :], in0=e4[:], scalar1=sm[:, 2:3],
                                                  scalar2=None, op0=ALU.mult),
                 reads=[Brt], writes=[Bg4], accumulate=True)
            for k in range(4):
                P.op("dve", lambda e: e.tensor_scalar(out=oh[:], in0=lg[:], scalar1=m8[:, k:k + 1],
                                                      scalar2=None, op0=ALU.is_equal),
                     reads=[Brt], writes=[Brt], accumulate=True)
                P.op("dve", lambda e: e.tensor_tensor(out=junk[:], in0=oh[:], in1=sbf[:], op=ALU.mult),
                     reads=[Brt], writes=[Brt], accumulate=True)
                P.op("dve", lambda e: e.tensor_reduce(out=slotf[:, k:k + 1], in_=junk[:], axis=AX.X,
                                                      op=ALU.add),
                     reads=[Brt], writes=[Brt], accumulate=True)
                if k == 0:
                    P.op("dve", lambda e: e.tensor_scalar(out=Gd[:], in0=oh[:], scalar1=g4all[:, i, 0:1],
                                                          scalar2=None, op0=ALU.mult),
                         reads=[Brt, Bg4], writes=[Brt], accumulate=True)
                else:
                    P.op("dve", lambda e: e.scalar_tensor_tensor(out=Gd[:], in0=oh[:],
                                                                 scalar=g4all[:, i, k:k + 1], in1=Gd[:],
                                                                 op0=ALU.mult, op1=ALU.add),
                         reads=[Brt, Bg4], writes=[Brt], accumulate=True)
            P.op("dve", lambda e: e.tensor_copy(out=idxall[:, i, :], in_=slotf[:]), reads=[Brt], writes=[Bidx],
                 accumulate=True)
            P.op("pe", lambda e: e.transpose(C.pb[4][0:32, 128:256], Gd[:], C.ident_f[:]),
                 reads=[Brt, C.Bconst], writes=[C.Bpb[4]])
            P.op("act", lambda e: e.copy(out=GT[:, i, :], in_=C.pb[4][0:32, 128:256]),
                 reads=[C.Bpb[4]], writes=[BGT], accumulate=True)
            for k in range(4):
                P.dma("pool", None, None, Bx1b, reads=[Bx1b, Bidx], writes=[Bxe], accumulate=True,
                      group=("sc", i),
                      fn=lambda e: e.indirect_dma_start(
                          out=xe_d[:, :], out_offset=bass.IndirectOffsetOnAxis(ap=idxall[:, i, k:k + 1], axis=0),
                          in_=x1b[:, :], in_offset=None))
    P.barrier()

    with ExitStack() as es:
        T = lambda name, shape, dt: es.enter_context(nc.sbuf_tensor("s_" + name, shape, dt))
        xes = [T(f"xes{i}", [128, 3, 1024], BF16) for i in range(2)]
        xeT = [T(f"xeT{i}", [128, 8 * CAP], BF16) for i in range(2)]
        hT = [T(f"hT{i}", [128, 8, CAP], BF16) for i in range(2)]
        yst = [T(f"yst{i}", [128, 1024], F32) for i in range(2)]
        ga = [T(f"ga{i}", [128, CAP], F32) for i in range(2)]
        sg = [T(f"sg{i}", [128, CAP], F32) for i in range(2)]
        ua = [T(f"ua{i}", [128, CAP], F32) for i in range(2)]
        tt = [T(f"tt{i}", [128, CAP], F32) for i in range(2)]
        Bxes = [P.buf(f"xes{i}") for i in range(2)]
        BxeT = [P.buf(f"xeT{i}") for i in range(2)]
        BhT = [P.buf(f"hT{i}") for i in range(2)]
        Byst = [P.buf(f"yst{i}") for i in range(2)]
        Bga = [P.buf(f"ga{i}") for i in range(2)]
        Bsg = [P.buf(f"sg{i}") for i in range(2)]
        Bua = [P.buf(f"ua{i}") for i in range(2)]
        Btt = [P.buf(f"tt{i}") for i in range(2)]
        bgu3 = bgu[:].rearrange("p (e c) -> p e c", c=16)
        psT = C.ps_all[:, 512 * 6:512 * 8].bitcast(BF16)
        ycnt = [0]
        qcnt = [0]

        def load_xe(e):
            P.dma("sp", xes[e % 2][:], xe_d[e * CAP:(e + 1) * CAP, :].rearrange("(a p) d -> p a d", p=128),
                  Bxes[e % 2], reads=[Bxe], writes=[Bxes[e % 2]])

        def transpose_e(e):
            xs, xt_ = xes[e % 2], xeT[e % 2]
            for rnd, (b0, b1) in enumerate(((0, 16), (16, 24))):
                banks = [C.Bpb[6], C.Bpb[7]] if rnd == 0 else [C.Bpb[6]]
                for blk in range(b0, b1):
                    c, a = blk // 3, blk % 3
                    o = (blk - b0) * 128
                    P.op("pe", lambda e_: e_.transpose(psT[:, o:o + 128], xs[:, a, 128 * c:128 * c + 128],
                                                       C.ident_b[:]),
                         reads=[Bxes[e % 2], C.Bconst], writes=banks, accumulate=(blk != b0))
                n = (b1 - b0) * 128
                P.op("act", lambda e_: e_.copy(out=xt_[:, b0 * 128:b0 * 128 + n], in_=psT[:, 0:n]),
                     reads=banks, writes=[BxeT[e % 2]], accumulate=(rnd != 0))

        def gu_e(e):
            xt_ = xeT[e % 2][:].rearrange("p (c s) -> p c s", s=CAP)
            for q in range(4):
                s = (4 * e + q) % 4
                for sub in range(2):
                    ch = 2 * q + sub
                    pp = qcnt[0] % 2
                    qcnt[0] += 1
                    bg_, bu_ = C.pb[2 * pp], C.pb[2 * pp + 1]
                    for two, bank in ((0, 2 * pp), (1, 2 * pp + 1)):
                        for d in range(8):
                            P.op("pe", lambda e_: e_.matmul(C.pb[bank][:, 0:CAP],
                                                            hi(gur[s][:, d, two, 128 * sub:128 * sub + 128]),
                                                            xt_[:, d, :], start=(d == 0), stop=(d == 7)),
                                 reads=[Bgur[s], BxeT[e % 2]], writes=[C.Bpb[bank]], accumulate=(d != 0))
                    P.op("dve", lambda e_: e_.tensor_scalar(out=ga[pp][:], in0=bg_[:, 0:CAP],
                                                            scalar1=bgu3[:, e, ch:ch + 1], scalar2=7.0,
                                                            op0=ALU.add, op1=ALU.min),
                         reads=[C.Bpb[2 * pp], Bbgu], writes=[Bga[pp]])
                    P.op("act", lambda e_: e_.activation(out=sg[pp][:], in_=ga[pp][:], func=AF.Sigmoid,
                                                         scale=1.702),
                         reads=[Bga[pp]], writes=[Bsg[pp]])
                    P.op("dve", lambda e_: e_.tensor_scalar(out=ua[pp][:], in0=bu_[:, 0:CAP],
                                                            scalar1=bgu3[:, e, 8 + ch:9 + ch], scalar2=8.0,
                                                            op0=ALU.add, op1=ALU.min),
                         reads=[C.Bpb[2 * pp + 1], Bbgu], writes=[Bua[pp]])
                    P.op("pool", lambda e_: e_.tensor_tensor(out=tt[pp][:], in0=ga[pp][:], in1=sg[pp][:],
                                                             op=ALU.mult),
                         reads=[Bga[pp], Bsg[pp]], writes=[Btt[pp]])
                    P.op("dve", lambda e_: e_.scalar_tensor_tensor(out=hT[e % 2][:, ch, :], in0=ua[pp][:],
                                                                   scalar=-6.0, in1=tt[pp][:],
                                                                   op0=ALU.max, op1=ALU.mult),
                         reads=[Bua[pp], Btt[pp]], writes=[BhT[e % 2]], accumulate=(ch != 0))
                load_gu()

        def dn_e(e):
            for st_ in range(3):
                ys = ycnt[0] % 2
                ycnt[0] += 1
                for hf in range(2):
                    s = (2 * e + hf) % 2
                    bank = 4 + hf
                    for f in range(8):
                        P.op("pe", lambda e_: e_.matmul(C.pb[bank], hT[e % 2][:, f, 128 * st_:128 * st_ + 128],
                                                        hi(dnr[s][:, f, :]), start=(f == 0), stop=(f == 7)),
                             reads=[BhT[e % 2], Bdnr[s]], writes=[C.Bpb[bank]], accumulate=(f != 0))
                    P.op("act", lambda e_: e_.copy(out=yst[ys][:, 512 * hf:512 * hf + 512], in_=C.pb[bank]),
                         reads=[C.Bpb[bank]], writes=[Byst[ys]], accumulate=(hf != 0))
                P.dma("act", ye_d[e * CAP + 128 * st_:e * CAP + 128 * st_ + 128, :], yst[ys][:], Byst[ys],
                      reads=[Byst[ys]], writes=[Bye], accumulate=True)
            load_dn()
            load_dn()

        load_xe(0)
        load_xe(1)
        transpose_e(0)
        gu_e(0)
        for e in range(NE):
            if e + 1 < NE:
                transpose_e(e + 1)
                if e + 2 < NE:
                    load_xe(e + 2)
                gu_e(e + 1)
            dn_e(e)
    P.barrier()

    with ExitStack() as es:
        T = lambda name, shape, dt: es.enter_context(nc.sbuf_tensor("s_" + name, shape, dt))
        yk = [[T(f"yk{j}_{k}", [128, 1024], F32) for k in range(4)] for j in range(2)]
        Byk = [[P.buf(f"yk{j}_{k}") for k in range(4)] for j in range(2)]
        x1r = [T(f"x1r{j}", [128, 1024], F32) for j in range(2)]
        Bx1r = [P.buf(f"x1r{j}") for j in range(2)]
        acc = T("acc", [128, 1024], F32)
        Bacc = P.buf("acc")
        outs = [T(f"outt{j}", [128, 1024], F32) for j in range(2)]
        Bouts = [P.buf(f"outt{j}") for j in range(2)]
        lnsc = dict(st=T("lnst2", [128, 2, 6], F32), mv=T("lnmv2", [128, 2], F32),
                    sd=T("lnsd2", [128, 2], F32), B=P.buf("lnsc2"))

        def fetch(i):
            j = i % 2
            for k in range(4):
                P.dma("pool", None, None, Byk[j][k], reads=[Bye, Bidx], writes=[Byk[j][k]],
                      fn=lambda e: e.indirect_dma_start(
                          out=yk[j][k][:, :], out_offset=None, in_=ye_d[:, :],
                          in_offset=bass.IndirectOffsetOnAxis(ap=idxall[:, i, k:k + 1], axis=0)))
            P.dma("sp", x1r[j][:], x1f_d[128 * i:128 * i + 128, :], Bx1r[j], reads=[Bx1f], writes=[Bx1r[j]])

        fetch(0)
        for i in range(NT):
            if i + 1 < NT:
                fetch(i + 1)
            j = i % 2
            for hf in range(2):
                P.op("pe", lambda e: e.matmul(C.pb[hf], GT[:, i, :], bdn[:, 512 * hf:512 * hf + 512],
                                              start=True, stop=True),
                     reads=[BGT, Bbdn], writes=[C.Bpb[hf]])
                P.op("dve", lambda e: e.scalar_tensor_tensor(out=acc[:, 512 * hf:512 * hf + 512],
                                                             in0=yk[j][0][:, 512 * hf:512 * hf + 512],
                                                             scalar=g4all[:, i, 0:1], in1=C.pb[hf],
                                                             op0=ALU.mult, op1=ALU.add),
                     reads=[Byk[j][0], Bg4, C.Bpb[hf]], writes=[Bacc], accumulate=(hf != 0))
            for k in range(1, 4):
                P.op("dve", lambda e: e.scalar_tensor_tensor(out=acc[:], in0=yk[j][k][:],
                                                             scalar=g4all[:, i, k:k + 1], in1=acc[:],
                                                             op0=ALU.mult, op1=ALU.add),
                     reads=[Byk[j][k], Bg4, Bacc], writes=[Bacc])
            P.op("dve", lambda e: e.scalar_tensor_tensor(out=acc[:], in0=x1r[j][:], scalar=ALPHA, in1=acc[:],
                                                         op0=ALU.mult, op1=ALU.add),
                 reads=[Bx1r[j], Bacc], writes=[Bacc])
            layer_norm_tile(C, acc, Bacc, lngb[:, 2, :], lngb[:, 3, :], Blngb, outs[j], Bouts[j], lnsc)
            P.dma("sp", xo[128 * i:128 * i + 128, :], outs[j][:], Bouts[j], reads=[Bouts[j]], writes=[Bxo],
                  accumulate=True)
    P.barrier()


def build_token():
    nc = bass.Bass("TRN2", target_bir_lowering=False)
    P = Prog(nc)
    io = {}
    io["xres"] = nc.dram_tensor("xres", [2048, D], F32, kind="ExternalInput").ap()
    io["OTall"] = nc.dram_tensor("OTall", [D, 2048], BF16, kind="ExternalInput").ap()
    io["wout"] = nc.dram_tensor("wout", [D, D], F32, kind="ExternalInput").ap()
    io["lngb"] = nc.dram_tensor("lngb", [128, 4, D], F32, kind="ExternalInput").ap()
    io["w_r"] = nc.dram_tensor("w_r", [D, NE], F32, kind="ExternalInput").ap()
    io["b_r"] = nc.dram_tensor("b_r", [128, NE], F32, kind="ExternalInput").ap()
    io["w_gu"] = nc.dram_tensor("w_gu", [NE, D, 2 * D], F32, kind="ExternalInput").ap()
    io["b_gu"] = nc.dram_tensor("b_gu", [NE, 2 * D], F32, kind="ExternalInput").ap()
    io["w_dn"] = nc.dram_tensor("w_dn", [NE, D, D], F32, kind="ExternalInput").ap()
    io["b_dn"] = nc.dram_tensor("b_dn", [NE, D], F32, kind="ExternalInput").ap()
    io["xo"] = nc.dram_tensor("xo", [2048, D], F32, kind="ExternalOutput").ap()
    io["xe"] = nc.dram_tensor("xe_scr", [NE * CAP, D], BF16, kind="Internal").ap()
    io["ye"] = nc.dram_tensor("ye_scr", [NE * CAP, D], F32, kind="Internal").ap()
    io["x1f"] = nc.dram_tensor("x1f_scr", [2048, D], F32, kind="Internal").ap()
    io["Bxo"] = P.buf("xo")
    C = Ctx(nc, P)
    phase_token(C, io)
    P.finish([io["Bxo"]])
    return nc


RMS_EPS = 1e-6
TWO_PI = 2.0 * math.pi


def phase_mla(C, io):
    from contextlib import ExitStack
    nc, P = C.nc, C.P
    xT, OT = io["xT"], io["OT"]
    lat_d, cq_d = io["lat_scr"], io["cq_scr"]
    Blat_d, Bcq_d = P.buf("latd"), P.buf("cqd")
    NB = S // 512
    alloc_attn_common(C)
    r = lambda ap: ap.rearrange("(c p) n -> p c n", p=128)
    SC = 96 ** -0.5

    QT = nc.alloc_sbuf_tensor("s_QT", [128, S], BF16)
    KT = nc.alloc_sbuf_tensor("s_KT", [128, S], BF16)
    VA = nc.alloc_sbuf_tensor("s_VA", [128, 64, 128], BF16)
    MQT = nc.alloc_sbuf_tensor("s_MQT", [64, S], BF16)
    MKT = nc.alloc_sbuf_tensor("s_MKT", [64, 256], BF16)
    MVA = nc.alloc_sbuf_tensor("s_MVA", [128, 2, 128], BF16)
    BQT, BKT, BVA, BMQT, BMK, BMV = [P.buf(n) for n in ("QT", "KT", "VA", "MQT", "MKT", "MVA")]
    cosT = nc.alloc_sbuf_tensor("s_cosT", [128, 64, 16], F32)
    sinT = nc.alloc_sbuf_tensor("s_sinT", [128, 64, 16], F32)
    krope = nc.alloc_sbuf_tensor("s_krope", [128, 64, 32], BF16)
    rstd_kv = nc.alloc_sbuf_tensor("s_rstdkv", [128, 64], F32)
    rstd_q = nc.alloc_sbuf_tensor("s_rstdq", [128, 64], F32)
    Brope, Bkr, Brs = P.buf("rope"), P.buf("krope"), P.buf("rstd")
    wukv = nc.alloc_sbuf_tensor("s_wukv", [128, 2, 3, 128], BF16)
    wuq = nc.alloc_sbuf_tensor("s_wuq", [128, 3, 288], BF16)
    Bwu = P.buf("wu")
    P.op("pool", lambda e: e.memset(VA[:, :, 64:128], 1.0), writes=[BVA])
    P.op("pool", lambda e: e.memset(MVA[:, :, 64:128], 1.0), writes=[BMV])

    with ExitStack() as es:
        T = lambda name, shape, dt: es.enter_context(nc.sbuf_tensor("s_" + name, shape, dt))
        wdkv = T("wdkv", [128, 8, 288], F32)
        wcq = T("wcq", [128, 8, 384], F32)
        wmq = T("wmq", [128, 8, 64], F32)
        wmk = T("wmk", [128, 8, 64], F32)
        wmv = T("wmv", [128, 8, 64], F32)
        memf = T("memf", [128, 8, 256], F32)
        memb = T("memb", [128, 8, 256], BF16)
        wuk_f = T("wuk_f", [128, 2, 192], F32)
        wuv_f = T("wuv_f", [128, 2, 192], F32)
        wuq_f = T("wuq_f", [128, 3, 288], F32)
        gkv = T("gkv", [128, 2], F32)
        gq = T("gq", [128, 3], F32)
        es_r = ExitStack()
        TR = lambda name, shape, dt: es_r.enter_context(nc.sbuf_tensor("s_" + name, shape, dt))
        posi = TR("posi", [64, 128], I32)
        posf = TR("posf", [64, 128], F32)
        posT = TR("posT", [128, 64], F32)
        invf = TR("invf", [128, 16], F32)
        ang = TR("ang", [128, 64, 16], F32)
        kq = TR("kq", [128, 64, 16], F32)
        ki = TR("ki", [128, 64, 16], I32)
        wr1 = TR("wr1", [128, 64, 16], F32)
        Bw = [P.buf(f"w{i}") for i in range(4)]
        Bmem, Bmemb, Bpos, Bsq, Brt, Brp = (P.buf("mem"), P.buf("memb"), P.buf("pos"), P.buf("sq"),
                                            P.buf("rtmp"), P.buf("rp"))
        Bxf = [P.buf(f"xsf{i}") for i in range(2)]
        Bxb = [P.buf(f"xsb{i}") for i in range(2)]
        Blat = [P.buf(f"latblk{i}") for i in range(2)]
        Bcq = [P.buf(f"cqblk{i}") for i in range(2)]

        P.dma("sp", wdkv[:], r(io["wdkv"]), Bw[0], writes=[Bw[0]])
        P.dma("sp", wcq[:], r(io["wcq"]), Bw[1], writes=[Bw[1]])
        P.dma("sp", wmq[:], r(io["wmq"]), Bw[2], writes=[Bw[2]], group="w", accumulate=True)
        P.dma("sp", wmk[:], r(io["wmk"]), Bw[2], writes=[Bw[2]], group="w", accumulate=True)
        P.dma("sp", wmv[:], r(io["wmv"]), Bw[2], writes=[Bw[2]], group="w", accumulate=True)
        P.dma("sp", wuk_f[:], r(io["wuk"]), Bw[3], writes=[Bw[3]], group="w", accumulate=True)
        P.dma("sp", wuv_f[:], r(io["wuv"]), Bw[3], writes=[Bw[3]], group="w", accumulate=True)
        P.dma("sp", wuq_f[:], r(io["wuq"]), Bw[3], writes=[Bw[3]], group="w", accumulate=True)
        with nc.allow_non_contiguous_dma(reason="tiny gain vectors"):
            P.dma("sp", gkv[:], io["gkv"].rearrange("(j p) -> p j", p=128), Bw[3], writes=[Bw[3]],
                  group="w", accumulate=True)
            P.dma("sp", gq[:], io["gq"].rearrange("(j p) -> p j", p=128), Bw[3], writes=[Bw[3]],
                  group="w", accumulate=True)
        P.dma("sp", memf[:], r(io["memT"]), Bmem, writes=[Bmem])
        P.dma("sp", posi[:], io["pos"].rearrange("(b t) -> b t", t=128), Bpos, writes=[Bpos])
        P.op("pool", lambda e: e.tensor_copy(out=memb[:], in_=memf[:]), reads=[Bmem], writes=[Bmemb])
        for j in range(2):
            for h in range(3):
                P.op("dve", lambda e: e.tensor_scalar(out=wukv[:, j, h, 0:64], in0=wuk_f[:, j, 64 * h:64 * h + 64],
                                                      scalar1=gkv[:, j:j + 1], scalar2=None, op0=ALU.mult),
                     reads=[Bw[3]], writes=[Bwu], accumulate=True)
                P.op("dve", lambda e: e.tensor_scalar(out=wukv[:, j, h, 64:128], in0=wuv_f[:, j, 64 * h:64 * h + 64],
                                                      scalar1=gkv[:, j:j + 1], scalar2=None, op0=ALU.mult),
                     reads=[Bw[3]], writes=[Bwu], accumulate=True)
        for j in range(3):
            P.op("dve", lambda e: e.tensor_scalar(out=wuq[:, j, :], in0=wuq_f[:, j, :], scalar1=gq[:, j:j + 1],
                                                  scalar2=None, op0=ALU.mult),
                 reads=[Bw[3]], writes=[Bwu], accumulate=True)

        P.op("dve", lambda e: e.tensor_copy(out=posf[:], in_=posi[:]), reads=[Bpos], writes=[Bpos])
        P.op("pe", lambda e: e.transpose(C.pb[5][:, 0:64], posf[:], C.ident_f[0:64, 0:64]),
             reads=[Bpos, C.Bconst], writes=[C.Bpb[5]])
        P.op("dve", lambda e: e.tensor_copy(out=posT[:], in_=C.pb[5][:, 0:64]), reads=[C.Bpb[5]], writes=[Brope])
        for f in range(16):
            val = float(np.float32(10000.0) ** np.float32(-f * 2.0 / 32.0))
            P.op("pool", lambda e: e.memset(invf[:, f:f + 1], val), writes=[Brope], accumulate=True)
        for f in range(16):
            P.op("dve", lambda e: e.tensor_scalar(out=ang[:, :, f], in0=posT[:], scalar1=invf[:, f:f + 1],
                                                  scalar2=None, op0=ALU.mult),
                 reads=[Brope], writes=[Brope], accumulate=True)
        C1 = 6.28125
        C2 = TWO_PI - C1
        P.op("dve", lambda e: e.tensor_scalar(out=kq[:], in0=ang[:], scalar1=1.0 / TWO_PI, scalar2=None,
                                              op0=ALU.mult), reads=[Brope], writes=[Brope], accumulate=True)
        P.op("dve", lambda e: e.tensor_copy(out=ki[:], in_=kq[:]), reads=[Brope], writes=[Brope], accumulate=True)
        P.op("dve", lambda e: e.tensor_copy(out=kq[:], in_=ki[:]), reads=[Brope], writes=[Brope], accumulate=True)
        P.op("dve", lambda e: e.scalar_tensor_tensor(out=ang[:], in0=kq[:], scalar=-C1, in1=ang[:],
                                                     op0=ALU.mult, op1=ALU.add),
             reads=[Brope], writes=[Brope], accumulate=True)
        P.op("dve", lambda e: e.scalar_tensor_tensor(out=ang[:], in0=kq[:], scalar=-C2, in1=ang[:],
                                                     op0=ALU.mult, op1=ALU.add),
             reads=[Brope], writes=[Brope], accumulate=True)

        def wrapped_sin(dst, shift):
            P.op("dve", lambda e: e.tensor_scalar(out=kq[:], in0=ang[:], scalar1=shift, scalar2=None,
                                                  op0=ALU.add), reads=[Brope], writes=[Brope], accumulate=True)
            P.op("dve", lambda e: e.tensor_scalar(out=wr1[:], in0=kq[:], scalar1=math.pi, scalar2=-TWO_PI,
                                                  op0=ALU.is_gt, op1=ALU.mult),
                 reads=[Brope], writes=[Brope], accumulate=True)
            P.op("dve", lambda e: e.tensor_tensor(out=kq[:], in0=kq[:], in1=wr1[:], op=ALU.add),
                 reads=[Brope], writes=[Brope], accumulate=True)
            P.op("dve", lambda e: e.tensor_scalar(out=wr1[:], in0=kq[:], scalar1=-math.pi, scalar2=TWO_PI,
                                                  op0=ALU.is_lt, op1=ALU.mult),
                 reads=[Brope], writes=[Brope], accumulate=True)
            P.op("dve", lambda e: e.tensor_tensor(out=kq[:], in0=kq[:], in1=wr1[:], op=ALU.add),
                 reads=[Brope], writes=[Brope], accumulate=True)
            P.op("dve", lambda e: e.tensor_scalar(out=kq[:], in0=kq[:], scalar1=math.pi, scalar2=-math.pi,
                                                  op0=ALU.min, op1=ALU.max),
                 reads=[Brope], writes=[Brope], accumulate=True)
            P.op("act", lambda e: e.activation(out=dst[:], in_=kq[:], func=AF.Sin),
                 reads=[Brope], writes=[Brope], accumulate=True)

        wrapped_sin(sinT, 0.0)
        wrapped_sin(cosT, math.pi / 2.0)
        P.barrier()
        es_r.close()
        if io.get("stop") == "rope":
            Bd = P.buf("dbg")
            P.dma("sp", io["dbg"][:, 0:1024], cosT[:].rearrange("p a b -> p (a b)"), Bd, reads=[Brope], writes=[io["BOT"]], accumulate=True)
            P.dma("sp", io["dbg"][:, 1024:2048], sinT[:].rearrange("p a b -> p (a b)"), Bd, reads=[Brope], writes=[io["BOT"]], accumulate=True, group="g")
            es.close()
            return
        xs_f = [T(f"xsf{i}", [128, 8, 512], F32) for i in range(2)]
        xs_b = [T(f"xsb{i}", [128, 8, 512], BF16) for i in range(2)]
        latblk = [T(f"latblk{i}", [128, 2, 512], BF16) for i in range(2)]
        cqblk = [T(f"cqblk{i}", [128, 3, 512], BF16) for i in range(2)]
        sq = [T(f"sq{i}", [128, 512], BF16) for i in range(5)]
        rtmp = T("rtmp", [128, 8], F32)
        rp = [T(f"rp{i}", [128, 4, 16], F32) for i in range(4)]

        for c in range(8):
            P.op("pe", lambda e: e.matmul(C.pb[5][0:64, 0:256], hi(wmk[:, c, :]), memb[:, c, :],
                                          start=(c == 0), stop=(c == 7)),
                 reads=[Bw[2], Bmemb], writes=[C.Bpb[5]], accumulate=(c != 0))
        P.op("act", lambda e: e.copy(out=MKT[:], in_=C.pb[5][0:64, 0:256]), reads=[C.Bpb[5]], writes=[BMK])
        for mb in range(2):
            for c in range(8):
                P.op("pe", lambda e: e.matmul(C.pb[6][:, 64 * mb:64 * mb + 64],
                                              memb[:, c, 128 * mb:128 * mb + 128], hi(wmv[:, c, :]),
                                              start=(c == 0), stop=(c == 7)),
                     reads=[Bw[2], Bmemb], writes=[C.Bpb[6]], accumulate=not (c == 0 and mb == 0))
        P.op("act", lambda e: e.copy(out=MVA[:, :, 0:64],
                                     in_=C.pb[6][:, 0:128].rearrange("p (a b) -> p a b", a=2)),
             reads=[C.Bpb[6]], writes=[BMV], accumulate=True)

        xTr = r(xT)
        seq = [0]
        lat_v = lat_d.rearrange("(j p) t -> p j t", p=128)
        cq_v = cq_d.rearrange("(j p) t -> p j t", p=128)

        def load_x(nb):
            i = seq[0] % 2
            seq[0] += 1
            P.dma("sp", xs_f[i][:], xTr[:, :, nb * 512:(nb + 1) * 512], Bxf[i], writes=[Bxf[i]])
            P.op("pool", lambda e: e.tensor_copy(out=xs_b[i][:], in_=xs_f[i][:]),
                 reads=[Bxf[i]], writes=[Bxb[i]])
            return i

        nxt = load_x(0)
        for nb in range(NB):
            i = nxt
            if nb + 1 < NB:
                nxt = load_x(nb + 1)
            bb = nb % 2
            cnt = 0
            for (wt, Bwt, nj, dst, Bdst, sq0) in ((wdkv, Bw[0], 2, latblk[bb], Blat[bb], 0),
                                                  (wcq, Bw[1], 3, cqblk[bb], Bcq[bb], 2)):
                for j in range(nj):
                    bank = cnt % 2
                    cnt += 1
                    for c in range(8):
                        P.op("pe", lambda e: e.matmul(C.pb[bank], hi(wt[:, c, 128 * j:128 * j + 128]),
                                                      xs_b[i][:, c, :], start=(c == 0), stop=(c == 7)),
                             reads=[Bwt, Bxb[i]], writes=[C.Bpb[bank]], accumulate=(c != 0))
                    P.op("act", lambda e: e.copy(out=dst[:, j, :], in_=C.pb[bank]),
                         reads=[C.Bpb[bank]], writes=[Bdst], accumulate=(j != 0))
                    P.op("act", lambda e: e.activation(out=sq[sq0 + j][:], in_=C.pb[bank], func=AF.Square),
                         reads=[C.Bpb[bank]], writes=[Bsq], accumulate=(sq0 + j != 0))
            P.dma("sp", lat_v[:, :, nb * 512:(nb + 1) * 512], latblk[bb][:], Blat[bb], reads=[Blat[bb]],
                  writes=[Blat_d], accumulate=True)
            P.dma("sp", cq_v[:, :, nb * 512:(nb + 1) * 512], cqblk[bb][:], Bcq[bb], reads=[Bcq[bb]],
                  writes=[Bcq_d], accumulate=True)
            first = True
            for (sq0, nj, col0) in ((0, 2, 0), (2, 3, 4)):
                for sb in range(4):
                    for j in range(nj):
                        P.op("pe", lambda e: e.matmul(C.pb[7][:, col0 + sb:col0 + sb + 1],
                                                      sq[sq0 + j][:, 128 * sb:128 * sb + 128], C.ones_b[:, 0:1],
                                                      start=(j == 0), stop=(j == nj - 1)),
                             reads=[Bsq, C.Bconst], writes=[C.Bpb[7]], accumulate=not first)
                        first = False
            P.op("dve", lambda e: e.tensor_scalar(out=rtmp[:, 0:4], in0=C.pb[7][:, 0:4], scalar1=1.0 / 256.0,
                                                  scalar2=RMS_EPS, op0=ALU.mult, op1=ALU.add),
                 reads=[C.Bpb[7]], writes=[Brt])
            P.op("dve", lambda e: e.tensor_scalar(out=rtmp[:, 4:8], in0=C.pb[7][:, 4:8], scalar1=1.0 / 384.0,
                                                  scalar2=RMS_EPS, op0=ALU.mult, op1=ALU.add),
                 reads=[C.Bpb[7]], writes=[Brt], accumulate=True)
            P.op("act", lambda e: e.activation(out=rtmp[:], in_=rtmp[:], func=AF.Sqrt),
                 reads=[Brt], writes=[Brt], accumulate=True)
            P.op("dve", lambda e: e.reciprocal(out=rstd_kv[:, 4 * nb:4 * nb + 4], in_=rtmp[:, 0:4]),
                 reads=[Brt], writes=[Brs], accumulate=True)
            P.op("dve", lambda e: e.reciprocal(out=rstd_q[:, 4 * nb:4 * nb + 4], in_=rtmp[:, 4:8]),
                 reads=[Brt], writes=[Brs], accumulate=True)
            for sb in range(4):
                for c in range(8):
                    P.op("pe", lambda e: e.matmul(C.pb[6][:, 32 * sb:32 * sb + 32],
                                                  xs_b[i][:, c, 128 * sb:128 * sb + 128],
                                                  hi(wdkv[:, c, 256:288]), start=(c == 0), stop=(c == 7)),
                         reads=[Bw[0], Bxb[i]], writes=[C.Bpb[6]], accumulate=not (c == 0 and sb == 0))
            kr = C.pb[6][:, 0:128].rearrange("p (a b) -> p a b", a=4)
            cs, sn = cosT[:, 4 * nb:4 * nb + 4, :], sinT[:, 4 * nb:4 * nb + 4, :]
            P.op("dve", lambda e: e.tensor_tensor(out=rp[0][:], in0=kr[:, :, 0:16], in1=cs, op=ALU.mult),
                 reads=[C.Bpb[6], Brope], writes=[Brp])
            P.op("dve", lambda e: e.tensor_tensor(out=rp[1][:], in0=kr[:, :, 16:32], in1=sn, op=ALU.mult),
                 reads=[C.Bpb[6], Brope], writes=[Brp], accumulate=True)
            P.op("dve", lambda e: e.tensor_tensor(out=rp[2][:], in0=kr[:, :, 0:16], in1=sn, op=ALU.mult),
                 reads=[C.Bpb[6], Brope], writes=[Brp], accumulate=True)
            P.op("dve", lambda e: e.tensor_tensor(out=rp[3][:], in0=kr[:, :, 16:32], in1=cs, op=ALU.mult),
                 reads=[C.Bpb[6], Brope], writes=[Brp], accumulate=True)
            P.op("dve", lambda e: e.tensor_tensor(out=krope[:, 4 * nb:4 * nb + 4, 0:16], in0=rp[0][:], in1=rp[1][:],
                                                  op=ALU.subtract),
                 reads=[Brp], writes=[Bkr], accumulate=True)
            P.op("dve", lambda e: e.tensor_tensor(out=krope[:, 4 * nb:4 * nb + 4, 16:32], in0=rp[2][:], in1=rp[3][:],
                                                  op=ALU.add),
                 reads=[Brp], writes=[Bkr], accumulate=True)
            for c in range(8):
                P.op("pe", lambda e: e.matmul(C.pb[5][0:64, :], hi(wmq[:, c, :]), xs_b[i][:, c, :],
                                              start=(c == 0), stop=(c == 7)),
                     reads=[Bw[2], Bxb[i]], writes=[C.Bpb[5]], accumulate=(c != 0))
            P.op("act", lambda e: e.copy(out=MQT[0:64, nb * 512:(nb + 1) * 512], in_=C.pb[5][0:64, :]),
                 reads=[C.Bpb[5]], writes=[BMQT], accumulate=True)
    P.barrier()
    if io.get("stop") == "pass0":
        Bd = P.buf("dbg")
        dbgt = nc.alloc_sbuf_tensor("s_dbgt", [128, 2048], F32)
        P.op("dve", lambda e: e.tensor_copy(out=dbgt[:, 0:64], in_=rstd_kv[:]), reads=[Brs], writes=[Bd])
        P.op("dve", lambda e: e.tensor_copy(out=dbgt[:, 64:128], in_=rstd_q[:]), reads=[Brs], writes=[Bd], accumulate=True)
        P.op("dve", lambda e: e.tensor_copy(out=dbgt[:, 128:128 + 1024], in_=krope[:, 0:32, :].rearrange("p a b -> p (a b)")), reads=[Bkr], writes=[Bd], accumulate=True)
        P.op("dve", lambda e: e.tensor_copy(out=dbgt[0:64, 1152:1152 + 512], in_=MQT[:, 0:512]), reads=[BMQT], writes=[Bd], accumulate=True)
        P.dma("sp", io["dbg"][:, 0:1664], dbgt[:, 0:1664], Bd, reads=[Bd], writes=[io["BOT"]], accumulate=True)
        return

    with ExitStack() as es:
        T = lambda name, shape, dt: es.enter_context(nc.sbuf_tensor("s_" + name, shape, dt))
        latb = [T(f"latb{i}", [128, 2, 512], BF16) for i in range(2)]
        cqb = [T(f"cqb{i}", [128, 3, 512], BF16) for i in range(2)]
        Blb = [P.buf(f"latb{i}") for i in range(2)]
        Bcb = [P.buf(f"cqb{i}") for i in range(2)]
        Ktok = T("Ktok", [128, 4, 128], BF16)
        Qf = T("Qf", [128, 4, 96], F32)
        Qtok = T("Qtok", [128, 4, 128], BF16)
        rq = [T(f"rq{i}", [128, 4, 16], F32) for i in range(4)]
        BKtok, BQf, BQtok, Brq = P.buf("Ktok"), P.buf("Qf"), P.buf("Qtok"), P.buf("rq")
        Bzp = P.buf("zpad")
        P.op("pool", lambda e: e.memset(Ktok[:], 0.0), writes=[Bzp])
        P.op("pool", lambda e: e.memset(Qtok[:], 0.0), writes=[Bzp], accumulate=True)
        psTb = [C.pb[2].bitcast(BF16), C.pb[3].bitcast(BF16)]
        lat_v = lat_d.rearrange("(j p) t -> p j t", p=128)
        cq_v = cq_d.rearrange("(j p) t -> p j t", p=128)
        seq2 = [0]

        def load_blk(nb):
            i = seq2[0] % 2
            seq2[0] += 1
            P.dma("sp", latb[i][:], lat_v[:, :, nb * 512:(nb + 1) * 512], Blb[i], reads=[Blat_d], writes=[Blb[i]])
            P.dma("sp", cqb[i][:], cq_v[:, :, nb * 512:(nb + 1) * 512], Bcb[i], reads=[Bcq_d], writes=[Bcb[i]])
            return i

        for h in range(3):
            nxt = load_blk(0)
            for nb in range(NB):
                i = nxt
                if nb + 1 < NB:
                    nxt = load_blk(nb + 1)
                for sb in range(4):
                    for j in range(2):
                        P.op("pe", lambda e: e.matmul(C.pb[0][:, 128 * sb:128 * sb + 128],
                                                      latb[i][:, j, 128 * sb:128 * sb + 128], wukv[:, j, h, :],
                                                      start=(j == 0), stop=(j == 1)),
                             reads=[Blb[i], Bwu], writes=[C.Bpb[0]], accumulate=not (j == 0 and sb == 0))
                kv = C.pb[0].rearrange("p (a b) -> p a b", a=4)
                for sb in range(4):
                    P.op("dve", lambda e: e.tensor_scalar(out=Ktok[:, sb, 0:64], in0=kv[:, sb, 0:64],
                                                          scalar1=rstd_kv[:, 4 * nb + sb:4 * nb + sb + 1],
                                                          scalar2=None, op0=ALU.mult),
                         reads=[C.Bpb[0], Brs], writes=[BKtok], accumulate=(sb != 0))
                    P.op("dve", lambda e: e.tensor_scalar(out=VA[:, 4 * nb + sb, 0:64], in0=kv[:, sb, 64:128],
                                                          scalar1=rstd_kv[:, 4 * nb + sb:4 * nb + sb + 1],
                                                          scalar2=None, op0=ALU.mult),
                         reads=[C.Bpb[0], Brs], writes=[BVA], accumulate=True)
                P.op("dve", lambda e: e.tensor_copy(out=Ktok[:, :, 64:96], in_=krope[:, 4 * nb:4 * nb + 4, :]),
                     reads=[Bkr], writes=[BKtok], accumulate=True)
                for sb in range(4):
                    P.op("pe", lambda e: e.transpose(psTb[0][:, 128 * sb:128 * sb + 128], Ktok[:, sb, :],
                                                     C.ident_b[:]),
                         reads=[BKtok, C.Bconst, Bzp], writes=[C.Bpb[2]], accumulate=(sb != 0))
                P.op("act", lambda e: e.copy(out=KT[:, nb * 512:(nb + 1) * 512], in_=psTb[0][:, 0:512]),
                     reads=[C.Bpb[2]], writes=[BKT], accumulate=True)
                for sb in range(4):
                    for j in range(3):
                        P.op("pe", lambda e: e.matmul(C.pb[1][:, 128 * sb:128 * sb + 96],
                                                      cqb[i][:, j, 128 * sb:128 * sb + 128],
                                                      wuq[:, j, 96 * h:96 * h + 96],
                                                      start=(j == 0), stop=(j == 2)),
                             reads=[Bcb[i], Bwu], writes=[C.Bpb[1]], accumulate=not (j == 0 and sb == 0))
                qv = C.pb[1].rearrange("p (a b) -> p a b", a=4)
                for sb in range(4):
                    P.op("dve", lambda e: e.tensor_scalar(out=Qf[:, sb, :], in0=qv[:, sb, 0:96],
                                                          scalar1=rstd_q[:, 4 * nb + sb:4 * nb + sb + 1],
                                                          scalar2=None, op0=ALU.mult),
                         reads=[C.Bpb[1], Brs], writes=[BQf], accumulate=(sb != 0))
                cs, sn = cosT[:, 4 * nb:4 * nb + 4, :], sinT[:, 4 * nb:4 * nb + 4, :]
                P.op("dve", lambda e: e.tensor_copy(out=Qtok[:, :, 0:64], in_=Qf[:, :, 0:64]),
                     reads=[BQf], writes=[BQtok])
                P.op("dve", lambda e: e.tensor_tensor(out=rq[0][:], in0=Qf[:, :, 64:80], in1=cs, op=ALU.mult),
                     reads=[BQf, Brope], writes=[Brq])
                P.op("dve", lambda e: e.tensor_tensor(out=rq[1][:], in0=Qf[:, :, 80:96], in1=sn, op=ALU.mult),
                     reads=[BQf, Brope], writes=[Brq], accumulate=True)
                P.op("dve", lambda e: e.tensor_tensor(out=rq[2][:], in0=Qf[:, :, 64:80], in1=sn, op=ALU.mult),
                     reads=[BQf, Brope], writes=[Brq], accumulate=True)
                P.op("dve", lambda e: e.tensor_tensor(out=rq[3][:], in0=Qf[:, :, 80:96], in1=cs, op=ALU.mult),
                     reads=[BQf, Brope], writes=[Brq], accumulate=True)
                P.op("dve", lambda e: e.tensor_tensor(out=Qtok[:, :, 64:80], in0=rq[0][:], in1=rq[1][:],
                                                      op=ALU.subtract),
                     reads=[Brq], writes=[BQtok], accumulate=True)
                P.op("dve", lambda e: e.tensor_tensor(out=Qtok[:, :, 80:96], in0=rq[2][:], in1=rq[3][:], op=ALU.add),
                     reads=[Brq], writes=[BQtok], accumulate=True)
                for sb in range(4):
                    P.op("pe", lambda e: e.transpose(psTb[1][:, 128 * sb:128 * sb + 128], Qtok[:, sb, :],
                                                     C.ident_b[:]),
                         reads=[BQtok, C.Bconst, Bzp], writes=[C.Bpb[3]], accumulate=(sb != 0))
                P.op("act", lambda e: e.copy(out=QT[:, nb * 512:(nb + 1) * 512], in_=psTb[1][:, 0:512]),
                     reads=[C.Bpb[3]], writes=[BQT], accumulate=True)

            if io.get("stop") == "head0":
                Bd = P.buf("dbg")
                dbgt = T("dbgt", [128, 2048], F32)
                P.op("dve", lambda e: e.memset(dbgt[:], 0.0), writes=[Bd])
                P.op("dve", lambda e: e.tensor_copy(out=dbgt[0:96, 0:512], in_=QT[0:96, 7680:8192]), reads=[BQT], writes=[Bd], accumulate=True)
                P.op("dve", lambda e: e.tensor_copy(out=dbgt[0:96, 512:1024], in_=KT[0:96, 7680:8192]), reads=[BKT], writes=[Bd], accumulate=True)
                P.op("dve", lambda e: e.tensor_copy(out=dbgt[:, 1024:1536], in_=VA[:, 60:64, :].rearrange("p a b -> p (a b)")), reads=[BVA], writes=[Bd], accumulate=True)
                P.dma("sp", io["dbg"][:, 0:1536], dbgt[:, 0:1536], Bd, reads=[Bd], writes=[io["BOT"]], accumulate=True)
                P.barrier()
                return
            items = [dict(K=128, QT=lambda I: QT[:, I * 512:(I + 1) * 512],
                          KT=lambda J: KT[:, J * 128:(J + 1) * 128],
                          VA=lambda J: VA[:, J, :], nkb=lambda I: 4 * I + 4, causal=True,
                          scale=SC, bias=None, reads=[BQT, BKT, BVA], row0=64 * h)]
            if h == 0:
                items.append(dict(K=64, QT=lambda I: MQT[0:64, I * 512:(I + 1) * 512],
                                  KT=lambda J: MKT[0:64, J * 128:(J + 1) * 128],
                                  VA=lambda J: MVA[:, J, :], nkb=lambda I: 2, causal=False,
                                  scale=0.125, bias=None, reads=[BMQT, BMK, BMV], row0=192))

            def cb(it, I, st_tile, st_buf):
                r0 = it["row0"]
                P.dma("sp", OT[r0:r0 + 64, I * 512:(I + 1) * 512], st_tile[0:64, :], st_buf,
                      reads=[st_buf], writes=[io["BOT"]], accumulate=True)
            attn_core(C, items, cb)
    P.barrier()


def build_mla(stop=None):
    nc = bass.Bass("TRN2", target_bir_lowering=False)
    P = Prog(nc)
    io = {"stop": stop}
    if stop:
        io["dbg"] = nc.dram_tensor("dbg", [128, 2048], F32, kind="ExternalOutput").ap()
    io["xT"] = nc.dram_tensor("xT", [D, S], F32, kind="ExternalInput").ap()
    for n, shp in (("wdkv", [D, 288]), ("wcq", [D, 384]), ("wmq", [D, 64]), ("wmk", [D, 64]), ("wmv", [D, 64]),
                   ("wuk", [256, 192]), ("wuv", [256, 192]), ("wuq", [384, 288]), ("memT", [D, 256]),
                   ("gkv", [256]), ("gq", [384])):
        io[n] = nc.dram_tensor(n, shp, F32, kind="ExternalInput").ap()
    io["pos"] = nc.dram_tensor("pos", [S], I32, kind="ExternalInput").ap()
    io["OT"] = nc.dram_tensor("OT", [256, S], BF16, kind="ExternalOutput").ap()
    io["lat_scr"] = nc.dram_tensor("lat_scr", [256, S], BF16, kind="Internal").ap()
    io["cq_scr"] = nc.dram_tensor("cq_scr", [384, S], BF16, kind="Internal").ap()
    io["BOT"] = P.buf("OT")
    C = Ctx(nc, P)
    phase_mla(C, io)
    P.finish([io["BOT"]])
    return nc


_PERM = np.concatenate([np.concatenate([np.arange(192 * g, 192 * g + 192),
                                        np.arange(768 + 64 * g, 768 + 64 * g + 64)]) for g in range(4)])
_PROGS = {}


def _prog(name, builder):
    if name not in _PROGS:
        _PROGS[name] = builder()
    return _PROGS[name]


def _c(a):
    return np.ascontiguousarray(a)


def _fox_maps(inp, x):
    w_in = inp["a_w_in"][0]
    wkv = inp["mem_w_kv"][0]
    maps = []
    for r in range(NCORES):
        b, g = r // 4, r % 4
        hs = slice(192 * g, 192 * g + 192)
        m = {"xT": _c(x[b].T),
             "wq": _c(w_in[:, 0:768][:, hs]), "wk": _c(w_in[:, 768:1536][:, hs]),
             "wv": _c(w_in[:, 1536:2304][:, hs]), "wf": _c(w_in[:, 2304 + 3 * g:2304 + 3 * g + 3]),
             "wmq": _c(w_in[:, 2316 + 64 * g:2316 + 64 * g + 64]),
             "wmk": _c(wkv[:, 64 * g:64 * g + 64]), "wmv": _c(wkv[:, 256 + 64 * g:256 + 64 * g + 64]),
             "memT": _c(inp["mem"][b].T),
             "nbf": _c(np.broadcast_to(inp["a_b_f"][0][3 * g:3 * g + 3][None, :], (128, 3)))}
        maps.append(m)
    return maps


def _mla_maps(inp, x):
    w_in = inp["b_w_in"][0]
    wkv = inp["mem_w_kv"][1]
    ukv = inp["kv_w_ukv"]
    uq = inp["b_w_uq"][0]
    maps = []
    for r in range(NCORES):
        b, g = r // 4, r % 4
        hs = [3 * g, 3 * g + 1, 3 * g + 2]
        m = {"xT": _c(x[b].T), "wdkv": _c(inp["kv_w_dkv"]), "wcq": _c(w_in[:, :384]),
             "wmq": _c(w_in[:, 384 + 64 * g:384 + 64 * g + 64]),
             "wmk": _c(wkv[:, 64 * g:64 * g + 64]), "wmv": _c(wkv[:, 256 + 64 * g:256 + 64 * g + 64]),
             "wuk": _c(np.concatenate([ukv[:, 128 * h:128 * h + 64] for h in hs], axis=1)),
             "wuv": _c(np.concatenate([ukv[:, 128 * h + 64:128 * h + 128] for h in hs], axis=1)),
             "wuq": _c(uq[:, 96 * hs[0]:96 * hs[0] + 288]),
             "memT": _c(inp["mem"][b].T), "gkv": _c(inp["kv_g"]), "gq": _c(inp["b_g_q"][0]),
             "pos": _c(inp["positions"][b].astype(np.int32))}
        maps.append(m)
    return maps


def _tok_maps(inp, l, xres_full, OT, wout):
    lngb = np.stack([inp["ln_g"][l, 0], inp["ln_b"][l, 0], inp["ln_g"][l, 1], inp["ln_b"][l, 1]])
    lngb = _c(np.broadcast_to(lngb[None], (128, 4, D)))
    wout_p = _c(wout[_PERM])
    b_r = _c(np.broadcast_to(inp["moe_b_r"][l][None], (128, NE)))
    maps = []
    for r in range(NCORES):
        b, j = r // 4, r % 4
        m = {"xres": _c(xres_full[b, 2048 * j:2048 * j + 2048]),
             "OTall": _c(np.concatenate([OT[4 * b + g][:, 2048 * j:2048 * j + 2048] for g in range(4)], axis=0)),
             "wout": wout_p, "lngb": lngb, "w_r": _c(inp["moe_w_r"][l]), "b_r": b_r,
             "w_gu": _c(inp["moe_w_gu"][l]), "b_gu": _c(inp["moe_b_gu"][l]),
             "w_dn": _c(inp["moe_w_dn"][l]), "b_dn": _c(inp["moe_b_dn"][l])}
        maps.append(m)
    return maps


def kernel(**inputs):
    inp = {k: np.asarray(v) for k, v in inputs.items()}
    cores = list(range(NCORES))
    x = inp["x"].astype(np.float32, copy=False)
    res = run_bass_kernel_spmd(_prog("fox", build_fox), _fox_maps(inp, x), core_ids=cores)
    OT0 = [np.asarray(r["OT"]) for r in res.results]
    res = run_bass_kernel_spmd(_prog("tok", build_token), _tok_maps(inp, 0, x, OT0, inp["a_w_out"][0]),
                               core_ids=cores)
    x1 = np.stack([np.asarray(r["xo"]) for r in res.results]).reshape(2, S, D)
    res = run_bass_kernel_spmd(_prog("mla", build_mla), _mla_maps(inp, x1), core_ids=cores)
    OT1 = [np.asarray(r["OT"]) for r in res.results]
    res = run_bass_kernel_spmd(_prog("tok", build_token), _tok_maps(inp, 1, x1, OT1, inp["b_w_out"][0]),
                               core_ids=cores)
    out = np.stack([np.asarray(r["xo"]) for r in res.results]).reshape(2, S, D)
    return out.astype(np.float32, copy=False)
```
